# Optimizing a Trainium2 kernel written in Bass

```python
import jax, jax.numpy as jnp
from jax import lax
import numpy as np

D_MODEL = 1024
BATCH = 8
SEQ = 4096
DEPTH = 4

CTX_LEN = 256
GRID_W = 64
N_MIXERS = 3
NORM_EPS = 1e-6
ROPE_THETA = 10000.0
Q_BLOCK = 128
FOURIER_GROUPS = 8
FOURIER_GROUP_DIM = D_MODEL // FOURIER_GROUPS
MLA_HEADS = 16
MLA_Q_LORA = 384
MLA_KV_LORA = 256
MLA_NOPE = 64
MLA_ROPE = 32
MLA_V = 64
GQA_HEADS = 16
GQA_KV_HEADS = 4
GQA_HEAD_DIM = 64
N_EXPERTS = 32
TOP_K = 4
D_EXPERT = 1024
SWIGLU_LIMIT = 7.0
SWIGLU_ALPHA = 1.702
EXPERT_BLOCK = 256
N_MOD = 6

kernel_name = "hybrid_fourier_mla_gqa_moe_dit"


def rms_norm(x, g):
    xf = x.astype(jnp.float32)
    y = xf * lax.rsqrt(jnp.mean(xf * xf, axis=-1, keepdims=True) + NORM_EPS)
    return (y * g.astype(jnp.float32)).astype(x.dtype)


def modulate(h, shift, scale):
    return h * (1 + scale) + shift


def axial_rope_angles(n_tok, rot_dim):
    rows = n_tok // GRID_W
    row = jnp.repeat(jnp.arange(rows, dtype=jnp.float32), GRID_W)
    col = jnp.tile(jnp.arange(GRID_W, dtype=jnp.float32), rows)
    n_freq = rot_dim // 4
    inv_freq = ROPE_THETA ** (-jnp.arange(n_freq, dtype=jnp.float32) / n_freq)
    ang = jnp.stack([row[:, None] * inv_freq, col[:, None] * inv_freq], axis=1)
    return jnp.cos(ang), jnp.sin(ang)


def apply_axial_rope(x, cos, sin):
    B, L, H, R = x.shape
    xf = x.astype(jnp.float32).reshape(B, L, H, 2, 2, R // 4)
    x1, x2 = xf[..., 0, :], xf[..., 1, :]
    cs, sn = cos[None, :, None], sin[None, :, None]
    out = jnp.stack([x1 * cs - x2 * sn, x2 * cs + x1 * sn], axis=-2)
    return out.reshape(B, L, H, R).astype(x.dtype)


def block_attention(q, k, v, scale):
    B, Lq, H, dk = q.shape
    Hkv, dv = k.shape[2], v.shape[3]
    grp = H // Hkv
    nb = Lq // Q_BLOCK
    kf = k.astype(jnp.float32)
    vf = v.astype(jnp.float32)
    qb = jnp.moveaxis(q.reshape(B, nb, Q_BLOCK, Hkv, grp, dk), 1, 0)

    def one_block(qblk):
        s = jnp.einsum("bqhgd,bkhd->bhgqk", qblk.astype(jnp.float32), kf) * scale
        p = jax.nn.softmax(s, axis=-1)
        return jnp.einsum("bhgqk,bkhd->bqhgd", p, vf)

    o = lax.map(one_block, qb)
    return jnp.moveaxis(o, 0, 1).reshape(B, Lq, H, dv).astype(q.dtype)


def fourier_mixer(h, w_in, w_out):
    B, L, D = h.shape
    u = (h @ w_in).astype(jnp.float32).reshape(B, L, FOURIER_GROUPS, FOURIER_GROUP_DIM)
    f = jnp.fft.fft2(u, axes=(1, 3), norm="ortho").real
    return f.reshape(B, L, D).astype(h.dtype) @ w_out


def mla_q(a, g_qa, w_qb, rope):
    B, L = a.shape[:2]
    q = (rms_norm(a[..., :MLA_Q_LORA], g_qa) @ w_qb).reshape(B, L, MLA_HEADS, MLA_NOPE + MLA_ROPE)
    q_nope, q_pe = q[..., :MLA_NOPE], q[..., MLA_NOPE:]
    if rope is not None:
        q_pe = apply_axial_rope(q_pe, rope[0], rope[1])
    return jnp.concatenate([q_nope, q_pe], axis=-1)


def mla_kv(a, g_kva, w_kvb, rope):
    B, L = a.shape[:2]
    c_kv = rms_norm(a[..., MLA_Q_LORA:MLA_Q_LORA + MLA_KV_LORA], g_kva)
    kv = (c_kv @ w_kvb).reshape(B, L, MLA_HEADS, MLA_NOPE + MLA_V)
    k_pe = a[..., MLA_Q_LORA + MLA_KV_LORA:][:, :, None, :]
    if rope is not None:
        k_pe = apply_axial_rope(k_pe, rope[0], rope[1])
    k = jnp.concatenate([kv[..., :MLA_NOPE], jnp.broadcast_to(k_pe, (B, L, MLA_HEADS, MLA_ROPE))], axis=-1)
    return k, kv[..., MLA_NOPE:]


def mla_mixer(h_ctx, h_lat, need_ctx_out, w_in, g_qa, w_qb, g_kva, w_kvb, w_o):
    B, L, _ = h_lat.shape
    rope = axial_rope_angles(L, MLA_ROPE)
    a_c = h_ctx @ w_in
    a_l = h_lat @ w_in
    k_c, v_c = mla_kv(a_c, g_kva, w_kvb, None)
    k_l, v_l = mla_kv(a_l, g_kva, w_kvb, rope)
    q_l = mla_q(a_l, g_qa, w_qb, rope)
    scale = (MLA_NOPE + MLA_ROPE) ** -0.5
    o_l = block_attention(q_l, jnp.concatenate([k_c, k_l], axis=1), jnp.concatenate([v_c, v_l], axis=1), scale)
    out_l = o_l.reshape(B, L, MLA_HEADS * MLA_V) @ w_o
    out_c = None
    if need_ctx_out:
        o_c = block_attention(mla_q(a_c, g_qa, w_qb, None), k_c, v_c, scale)
        out_c = o_c.reshape(B, h_ctx.shape[1], MLA_HEADS * MLA_V) @ w_o
    return out_c, out_l


GQA_NQ = GQA_HEADS * GQA_HEAD_DIM
GQA_NKV = GQA_KV_HEADS * GQA_HEAD_DIM


def gqa_q(a, g_q):
    B, L = a.shape[:2]
    return rms_norm(a[..., :GQA_NQ].reshape(B, L, GQA_HEADS, GQA_HEAD_DIM), g_q)


def gqa_kv(a, g_k):
    B, L = a.shape[:2]
    k = rms_norm(a[..., GQA_NQ:GQA_NQ + GQA_NKV].reshape(B, L, GQA_KV_HEADS, GQA_HEAD_DIM), g_k)
    v = a[..., GQA_NQ + GQA_NKV:].reshape(B, L, GQA_KV_HEADS, GQA_HEAD_DIM)
    return k, v


def gqa_mixer(h_ctx, h_lat, need_ctx_out, w_qkv, g_q, g_k, w_o):
    B, L, _ = h_lat.shape
    cos, sin = axial_rope_angles(L, GQA_HEAD_DIM)
    a_c = h_ctx @ w_qkv
    a_l = h_lat @ w_qkv
    k_c, v_c = gqa_kv(a_c, g_k)
    k_l, v_l = gqa_kv(a_l, g_k)
    k_l = apply_axial_rope(k_l, cos, sin)
    q_l = apply_axial_rope(gqa_q(a_l, g_q), cos, sin)
    scale = GQA_HEAD_DIM ** -0.5
    o_l = block_attention(q_l, jnp.concatenate([k_c, k_l], axis=1), jnp.concatenate([v_c, v_l], axis=1), scale)
    out_l = o_l.reshape(B, L, GQA_NQ) @ w_o
    out_c = None
    if need_ctx_out:
        o_c = block_attention(gqa_q(a_c, g_q), k_c, v_c, scale)
        out_c = o_c.reshape(B, h_ctx.shape[1], GQA_NQ) @ w_o
    return out_c, out_l


def moe_ffn(h, w_router, b_router, w_gu, b_gu, w_down, b_down):
    N, D = h.shape
    logits = h.astype(jnp.float32) @ w_router.astype(jnp.float32) + b_router.astype(jnp.float32)
    top_logit, top_idx = lax.top_k(logits, TOP_K)
    top_w = jax.nn.softmax(top_logit, axis=-1)
    n_assign = N * TOP_K
    flat_e = top_idx.reshape(-1)
    flat_tok = jnp.arange(n_assign, dtype=jnp.int32) // TOP_K
    flat_w = top_w.reshape(-1)
    order = jnp.argsort(flat_e)
    e_sorted = flat_e[order]
    counts = jnp.bincount(flat_e, length=N_EXPERTS)
    padded = (counts + EXPERT_BLOCK - 1) // EXPERT_BLOCK * EXPERT_BLOCK
    pad_end = jnp.cumsum(padded)
    pad_start = pad_end - padded
    grp_start = jnp.cumsum(counts) - counts
    rank = jnp.arange(n_assign, dtype=jnp.int32) - grp_start[e_sorted]
    dest = pad_start[e_sorted] + rank
    n_blocks = (n_assign + N_EXPERTS * (EXPERT_BLOCK - 1) + EXPERT_BLOCK - 1) // EXPERT_BLOCK
    n_slots = n_blocks * EXPERT_BLOCK
    slot_tok = jnp.full((n_slots,), N, jnp.int32).at[dest].set(flat_tok[order])
    slot_w = jnp.zeros((n_slots,), jnp.float32).at[dest].set(flat_w[order])
    block_start = jnp.arange(n_blocks, dtype=pad_end.dtype) * EXPERT_BLOCK
    block_expert = jnp.minimum(jnp.searchsorted(pad_end, block_start, side="right"), N_EXPERTS - 1)
    h_pad = jnp.concatenate([h, jnp.zeros((1, D), h.dtype)], axis=0)

    def expert_block(args):
        tok, wt, e = args
        gu = h_pad[tok] @ w_gu[e] + b_gu[e]
        gate = jnp.minimum(gu[..., ::2], SWIGLU_LIMIT)
        up = jnp.clip(gu[..., 1::2], -SWIGLU_LIMIT, SWIGLU_LIMIT)
        glu = gate * jax.nn.sigmoid(SWIGLU_ALPHA * gate)
        y = ((up + 1) * glu) @ w_down[e] + b_down[e]
        return y.astype(jnp.float32) * wt[:, None]

    y = lax.map(expert_block, (slot_tok.reshape(n_blocks, EXPERT_BLOCK),
                               slot_w.reshape(n_blocks, EXPERT_BLOCK), block_expert))
    out = jax.ops.segment_sum(y.reshape(n_slots, D), slot_tok, num_segments=N + 1)[:N]
    return out.astype(h.dtype)


def setup_inputs(seed: int = 0) -> dict:
    key = jax.random.key(seed)
    keys = jax.random.split(key, 32)
    ctr = iter(range(32))
    D = D_MODEL
    n_f = len(range(0, DEPTH, N_MIXERS))
    n_m = len(range(1, DEPTH, N_MIXERS))
    n_g = len(range(2, DEPTH, N_MIXERS))

    def nrm(shape, s):
        return jax.random.normal(keys[next(ctr)], shape, jnp.float32) * s

    def gain(shape):
        return 1.0 + nrm(shape, 0.02)

    return {
        "x": nrm((BATCH, SEQ, D), 1.0),
        "c": nrm((BATCH, D), 1.0),
        "ctx": nrm((BATCH, CTX_LEN, D), 1.0),
        "c_ctx": nrm((D,), 1.0),
        "w_mod": nrm((DEPTH, D, N_MOD * D), 0.5 * D ** -0.5),
        "b_mod": nrm((DEPTH, N_MOD * D), 0.01),
        "g_mix": gain((DEPTH, D)),
        "g_ffn": gain((DEPTH, D)),
        "g_final": gain((D,)),
        "f_w_in": nrm((n_f, D, D), D ** -0.5),
        "f_w_out": nrm((n_f, D, D), D ** -0.5),
        "mla_w_in": nrm((n_m, D, MLA_Q_LORA + MLA_KV_LORA + MLA_ROPE), D ** -0.5),
        "mla_g_qa": gain((n_m, MLA_Q_LORA)),
        "mla_w_qb": nrm((n_m, MLA_Q_LORA, MLA_HEADS * (MLA_NOPE + MLA_ROPE)), MLA_Q_LORA ** -0.5),
        "mla_g_kva": gain((n_m, MLA_KV_LORA)),
        "mla_w_kvb": nrm((n_m, MLA_KV_LORA, MLA_HEADS * (MLA_NOPE + MLA_V)), MLA_KV_LORA ** -0.5),
        "mla_w_o": nrm((n_m, MLA_HEADS * MLA_V, D), (MLA_HEADS * MLA_V) ** -0.5),
        "gqa_w_qkv": nrm((n_g, D, GQA_NQ + 2 * GQA_NKV), D ** -0.5),
        "gqa_g_q": gain((n_g, GQA_HEAD_DIM)),
        "gqa_g_k": gain((n_g, GQA_HEAD_DIM)),
        "gqa_w_o": nrm((n_g, GQA_NQ, D), GQA_NQ ** -0.5),
        "moe_w_router": nrm((DEPTH, D, N_EXPERTS), D ** -0.5),
        "moe_b_router": nrm((DEPTH, N_EXPERTS), 0.01),
        "moe_w_gu": nrm((DEPTH, N_EXPERTS, D, 2 * D_EXPERT), D ** -0.5),
        "moe_b_gu": nrm((DEPTH, N_EXPERTS, 2 * D_EXPERT), 0.01),
        "moe_w_down": nrm((DEPTH, N_EXPERTS, D_EXPERT, D), D_EXPERT ** -0.5),
        "moe_b_down": nrm((DEPTH, N_EXPERTS, D), 0.01),
    }


def reference(x, c, ctx, c_ctx, w_mod, b_mod, g_mix, g_ffn, g_final, f_w_in, f_w_out,
              mla_w_in, mla_g_qa, mla_w_qb, mla_g_kva, mla_w_kvb, mla_w_o,
              gqa_w_qkv, gqa_g_q, gqa_g_k, gqa_w_o,
              moe_w_router, moe_b_router, moe_w_gu, moe_b_gu, moe_w_down, moe_b_down):
    D = x.shape[-1]
    lat, cx = x, ctx
    for i in range(DEPTH):
        kind, j = i % N_MIXERS, i // N_MIXERS
        last = i == DEPTH - 1
        B, L, _ = lat.shape
        n_c = cx.shape[1]
        m_l = (jax.nn.silu(c) @ w_mod[i] + b_mod[i]).reshape(B, N_MOD, 1, D)
        h_l = modulate(rms_norm(lat, g_mix[i]), m_l[:, 0], m_l[:, 1])
        ctx_used = (kind != 0) or (not last)
        if ctx_used:
            m_c = (jax.nn.silu(c_ctx) @ w_mod[i] + b_mod[i]).reshape(N_MOD, D)
            h_c = modulate(rms_norm(cx, g_mix[i]), m_c[0], m_c[1])
        if kind == 0:
            o_l = fourier_mixer(h_l, f_w_in[j], f_w_out[j])
            o_c = None if last else fourier_mixer(h_c, f_w_in[j], f_w_out[j])
        elif kind == 1:
            o_c, o_l = mla_mixer(h_c, h_l, not last, mla_w_in[j], mla_g_qa[j], mla_w_qb[j],
                                 mla_g_kva[j], mla_w_kvb[j], mla_w_o[j])
        else:
            o_c, o_l = gqa_mixer(h_c, h_l, not last, gqa_w_qkv[j], gqa_g_q[j], gqa_g_k[j], gqa_w_o[j])
        lat = lat + m_l[:, 2] * o_l
        h2_l = modulate(rms_norm(lat, g_ffn[i]), m_l[:, 3], m_l[:, 4]).reshape(B * L, D)
        moe_p = (moe_w_router[i], moe_b_router[i], moe_w_gu[i], moe_b_gu[i], moe_w_down[i], moe_b_down[i])
        if last:
            f_l = moe_ffn(h2_l, *moe_p)
        else:
            cx = cx + m_c[2] * o_c
            h2_c = modulate(rms_norm(cx, g_ffn[i]), m_c[3], m_c[4]).reshape(B * n_c, D)
            f_all = moe_ffn(jnp.concatenate([h2_c, h2_l], axis=0), *moe_p)
            cx = cx + m_c[5] * f_all[:B * n_c].reshape(B, n_c, D)
            f_l = f_all[B * n_c:]
        lat = lat + m_l[:, 5] * f_l.reshape(B, L, D)
    return rms_norm(lat, g_final)
```

```python
import numpy as np
import concourse.bass as bass
import concourse.mybir as mybir
from concourse.bass_utils import run_bass_kernel_spmd

F32 = mybir.dt.float32
F32R = mybir.dt.float32r
BF16 = mybir.dt.bfloat16
ALU = mybir.AluOpType
AF = mybir.ActivationFunctionType
AX = mybir.AxisListType

P = 128
D = 1024
KC = 8
NE = 32
DEPTH = 4
CTX = 256
SEQ = 4096
EPS = 1e-6
SBUF_BASE = 16512
SBUF_CAP = 229376 - 128


class T:
    __slots__ = ("w", "r", "name")

    def __init__(self, name=""):
        self.w = None
        self.r = {}
        self.name = name


class Buf:
    __slots__ = ("ap", "t")

    def __init__(self, ap, name=""):
        self.ap = ap
        self.t = T(name)

    def __getitem__(self, k):
        return self.ap[k]


class Ring:
    def __init__(self, bufs):
        self.bufs = bufs
        self.i = 0

    def next(self):
        b = self.bufs[self.i % len(self.bufs)]
        self.i += 1
        return b


def _t(b):
    return b.t if isinstance(b, Buf) else b


class Ctx:
    N_DMA_SEMS = {"sp": 10, "pool": 8, "act": 2}

    def __init__(self, nc):
        self.nc = nc
        self.E = {"pe": nc.tensor, "dve": nc.vector, "act": nc.scalar, "pool": nc.gpsimd, "sp": nc.sync}
        self.sem = {}
        self.cnt = {}
        for e in ("pe", "dve", "act", "pool"):
            self.sem[e] = nc.alloc_semaphore("s_" + e)
            self.cnt[e] = 0
        self.dsems = {}
        self.drr = {}
        for q, n in self.N_DMA_SEMS.items():
            self.dsems[q] = []
            self.drr[q] = 0
            for i in range(n):
                nm = "d_%s%d" % (q, i)
                self.sem[nm] = nc.alloc_semaphore(nm)
                self.cnt[nm] = 0
                self.dsems[q].append(nm)
        self.seen = {e: {} for e in self.E}
        self.n_inst = 0
        self._id = 0
        self.pbase = SBUF_BASE
        self.abase = SBUF_BASE

    def _sb_at(self, shape, dtype, off, name):
        self._id += 1
        name = "%s_%d" % (name or "sb", self._id)
        h = self.nc.alloc_sbuf_tensor_at(name, list(shape), dtype, offset=off)
        return Buf(h, name)

    @staticmethod
    def _bytes(shape, dtype):
        n = 1
        for s in shape[1:]:
            n *= s
        n *= 2 if dtype == BF16 else 4
        return (n + 31) // 32 * 32

    def sbp(self, shape, dtype=F32, name=None):
        off = self.pbase
        self.pbase += self._bytes(shape, dtype)
        assert self.pbase <= SBUF_CAP, "persistent sbuf overflow"
        self.abase = max(self.abase, self.pbase)
        return self._sb_at(shape, dtype, off, name)

    def arena_reset(self):
        self.abase = self.pbase

    def sba(self, shape, dtype=F32, name=None):
        off = self.abase
        self.abase += self._bytes(shape, dtype)
        assert self.abase <= SBUF_CAP, "arena sbuf overflow %d" % self.abase
        return self._sb_at(shape, dtype, off, name)

    def ps(self, shape, name=None):
        self._id += 1
        name = "%s_%d" % (name or "ps", self._id)
        h = self.nc.alloc_psum_tensor(name, list(shape), F32)
        return Buf(h, name)

    def _deps(self, r, w):
        deps = {}
        for b in r:
            t = _t(b)
            if t.w is not None:
                s, v = t.w
                if deps.get(s, 0) < v:
                    deps[s] = v
        for b in w:
            t = _t(b)
            if t.w is not None:
                s, v = t.w
                if deps.get(s, 0) < v:
                    deps[s] = v
            for s, v in t.r.items():
                if deps.get(s, 0) < v:
                    deps[s] = v
        return deps

    def _wait(self, eng, deps):
        seen = self.seen[eng]
        e = self.E[eng]
        for s, v in deps.items():
            if s == "pe" and eng == "pe":
                continue
            if seen.get(s, 0) >= v:
                continue
            e.wait_ge(self.sem[s], v)
            seen[s] = v
            self.n_inst += 1

    def _mark(self, r, w, s, v):
        for b in w:
            t = _t(b)
            t.w = (s, v)
            t.r = {}
        for b in r:
            t = _t(b)
            if t.r.get(s, 0) < v:
                t.r[s] = v

    def op(self, eng, fn, r=(), w=()):
        self._wait(eng, self._deps(r, w))
        inst = fn()
        inst.then_inc(self.sem[eng], 1)
        self.cnt[eng] += 1
        self.n_inst += 1
        self._mark(r, w, eng, self.cnt[eng])
        return inst

    def dma(self, q, out, in_, r=(), w=(), **kw):
        lst = self.dsems[q]
        nm = lst[self.drr[q] % len(lst)]
        self.drr[q] += 1
        deps = self._deps(r, w)
        if self.cnt[nm] > 0:
            deps[nm] = max(deps.get(nm, 0), self.cnt[nm])
        self._wait(q, deps)
        inst = self.E[q].dma_start(out=out, in_=in_, **kw)
        self.cnt[nm] += 16
        inst.then_inc(self.sem[nm], 16)
        self.n_inst += 1
        self._mark(r, w, nm, self.cnt[nm])
        return inst

    def barrier(self):
        deps = {s: v for s, v in self.cnt.items() if v > 0}
        for e in self.E:
            self._wait(e, dict(deps))

    def finish(self):
        deps = {s: v for s, v in self.cnt.items() if v > 0}
        self._wait("sp", deps)


class Prog:
    def __init__(self, cfg):
        self.cfg = cfg
        nc = bass.Bass("TRN2", target_bir_lowering=False)
        self.nc = nc
        self.cx = Ctx(nc)
        self.din = {}
        self.dram_t = {}

    def inp(self, name, shape, dtype=F32):
        h = self.nc.dram_tensor(name, list(shape), dtype, kind="ExternalInput")
        self.din[name] = h.ap()
        return h.ap()

    def outp(self, name, shape):
        h = self.nc.dram_tensor(name, list(shape), F32, kind="ExternalOutput")
        return h.ap()

    def scratch(self, name, shape):
        h = self.nc.dram_tensor(name, list(shape), F32, kind="Internal")
        return h.ap()

    def dt(self, key):
        if key not in self.dram_t:
            self.dram_t[key] = T(str(key))
        return self.dram_t[key]

    def setup_common(self):
        cx = self.cx
        self.ident_d = self.inp("ident", [P, P])
        self.ones_d = self.inp("ones", [P, P])
        self.ident = cx.sbp([P, P], F32, "ident")
        self.ones = cx.sbp([P, P], F32, "ones")
        cx.dma("sp", self.ident[:], self.ident_d, w=[self.ident])
        cx.dma("sp", self.ones[:], self.ones_d, w=[self.ones])
        self.pa = Ring([cx.ps([P, 512], "pa%d" % i) for i in range(4)])
        self.py = Ring([cx.ps([P, 1024], "py%d" % i) for i in range(2)])

    def setup_mod(self):
        cx = self.cx
        self.csT_d = self.inp("csT", [P, KC, 2])
        self.w_mod_d = self.inp("w_mod", [DEPTH, D, 6 * D])
        self.b_modT_d = self.inp("b_modT", [DEPTH, P, 48])
        self.b_mod_d = self.inp("b_mod", [DEPTH, 1, 6 * D])
        self.g_mixT_d = self.inp("g_mixT", [DEPTH, P, KC])
        self.g_ffnT_d = self.inp("g_ffnT", [DEPTH, P, KC])
        self.sT = cx.sbp([P, KC, 2], F32, "sT")
        self.sB = [cx.sbp([P, KC, P], F32, "sB%d" % v) for v in range(2)]
        self.modT = cx.sbp([P, 48, 2], F32, "modT")
        self.AS = cx.sbp([P, 4, KC, 2], F32, "AS")
        self.gB = [[cx.sbp([P, D], F32, "gB%d%d" % (v, k)) for k in range(2)] for v in range(2)]
        self.bmT = cx.sbp([P, 48], F32, "bmT")
        self.gT = cx.sbp([P, 2, KC], F32, "gT")
        self.bmrow = cx.sbp([1, 2 * D], F32, "bmrow")
        cs = cx.sbp([P, KC, 2], F32, "cs")
        cx.dma("sp", cs[:], self.csT_d, w=[cs])
        cx.op("act", lambda: self.nc.scalar.activation(out=self.sT[:], in_=cs[:], func=AF.Silu),
              r=[cs], w=[self.sT])
        for v in range(2):
            cx.op("dve", lambda v=v: self.nc.vector.tensor_copy(
                self.sB[v][:], self.sT[:, :, v:v + 1].to_broadcast([P, KC, P])),
                r=[self.sT], w=[self.sB[v]])

    def stage_mod(self, l):
        cx, nc = self.cx, self.nc
        cx.barrier()
        cx.arena_reset()
        wring = Ring([cx.sba([P, KC, 256], F32, "wm%d" % i) for i in range(4)])
        cx.dma("sp", self.bmT[:], self.b_modT_d[l], w=[self.bmT])
        cx.dma("sp", self.bmrow[:, 0:D], self.b_mod_d[l][:, 2 * D:3 * D], w=[self.bmrow])
        cx.dma("sp", self.bmrow[:, D:2 * D], self.b_mod_d[l][:, 5 * D:6 * D], w=[self.bmrow])
        cx.dma("sp", self.gT[:, 0, :], self.g_mixT_d[l], w=[self.gT])
        cx.dma("sp", self.gT[:, 1, :], self.g_ffnT_d[l], w=[self.gT])
        wsrc = self.w_mod_d[l].rearrange("(kc p) n -> p kc n", p=P)
        for c in range(24):
            c0 = c * 256
            wch = wring.next()
            cx.dma("sp", wch[:], wsrc[:, :, c0:c0 + 256], w=[wch])
            pa = self.pa.next()
            for jj in range(2):
                j = 2 * c + jj
                for kc in range(KC):
                    cx.op("pe", lambda kc=kc, jj=jj: nc.tensor.matmul(
                        pa[:, jj * 2:jj * 2 + 2], wch[:, kc, jj * 128:(jj + 1) * 128], self.sT[:, kc, :],
                        start=(kc == 0), stop=(kc == KC - 1)), r=[wch, self.sT], w=[pa])
                cx.op("dve", lambda j=j, jj=jj: nc.vector.tensor_scalar(
                    out=self.modT[:, j, :], in0=pa[:, jj * 2:jj * 2 + 2], scalar1=self.bmT[:, j:j + 1],
                    scalar2=None, op0=ALU.add), r=[pa, self.bmT], w=[self.modT])
            m = c0 // D
            if m in (2, 5):
                k = 0 if m == 2 else 1
                cc0 = c0 - m * D
                for v in range(2):
                    pb = self.pa.next()
                    for kc in range(KC):
                        cx.op("pe", lambda kc=kc, v=v: nc.tensor.matmul(
                            pb[:, 0:256], self.sB[v][:, kc, :], wch[:, kc, :],
                            start=(kc == 0), stop=False), r=[wch, self.sB[v]], w=[pb])
                    cx.op("pe", lambda k=k, cc0=cc0: nc.tensor.matmul(
                        pb[:, 0:256], self.ones[0:1, :], self.bmrow[0:1, k * D + cc0:k * D + cc0 + 256],
                        start=False, stop=True), r=[self.ones, self.bmrow], w=[pb])
                    cx.op("act", lambda v=v, k=k, cc0=cc0: nc.scalar.copy(
                        out=self.gB[v][k][:, cc0:cc0 + 256], in_=pb[:, 0:256]), r=[pb], w=[self.gB[v][k]])
        for which, (mshift, mscale) in enumerate(((0, 1), (3, 4))):
            for v in range(2):
                cx.op("dve", lambda which=which, mscale=mscale, v=v: nc.vector.scalar_tensor_tensor(
                    out=self.AS[:, 2 * which, :, v], in0=self.modT[:, mscale * 8:mscale * 8 + 8, v],
                    scalar=1.0, in1=self.gT[:, which, :], op0=ALU.add, op1=ALU.mult),
                    r=[self.modT, self.gT], w=[self.AS])
                cx.op("dve", lambda which=which, mshift=mshift, v=v: nc.vector.tensor_copy(
                    self.AS[:, 2 * which + 1, :, v], self.modT[:, mshift * 8:mshift * 8 + 8, v]),
                    r=[self.modT], w=[self.AS])

    def rstd(self, ss, n=D):
        cx, nc = self.cx, self.nc
        cx.op("dve", lambda: nc.vector.tensor_scalar(
            out=ss[:, 1:2], in0=ss[:, 0:1], scalar1=1.0 / n, scalar2=EPS, op0=ALU.mult, op1=ALU.add),
            r=[ss], w=[ss])
        cx.op("act", lambda: nc.scalar.sqrt(out=ss[:, 1:2], in_=ss[:, 1:2]), r=[ss], w=[ss])
        cx.op("dve", lambda: nc.vector.reciprocal(out=ss[:, 2:3], in_=ss[:, 1:2]), r=[ss], w=[ss])

    def norm_mod_T(self, src_ap, src_t, which, v, dst, dst_t, dcol, xr, xnr, sst):
        cx, nc = self.cx, self.nc
        x = xr.next()
        cx.dma("sp", x[:], src_ap, r=[src_t], w=[x])
        xn = xnr.next()
        ss = sst.next()
        cx.op("dve", lambda: nc.vector.scalar_tensor_tensor(
            out=xn[:], in0=x[:], scalar=1.0, in1=x[:], op0=ALU.mult, op1=ALU.mult, accum_out=ss[:, 0:1]),
            r=[x], w=[xn, ss])
        self.rstd(ss)
        cx.op("act", lambda: nc.scalar.activation(out=xn[:], in_=x[:], func=AF.Identity, scale=ss[:, 2:3]),
              r=[x, ss], w=[xn])
        for hb in range(2):
            pa = self.pa.next()
            for cc in range(4):
                c = hb * 4 + cc
                cx.op("pe", lambda c=c, cc=cc: nc.tensor.transpose(
                    out=pa[:, cc * 128:(cc + 1) * 128], in_=xn[:, c * 128:(c + 1) * 128], identity=self.ident[:]),
                    r=[xn, self.ident], w=[pa])
            for cc in range(4):
                c = hb * 4 + cc
                a_ap = self.AS[:, 2 * which, c, v:v + 1]
                s_ap = self.AS[:, 2 * which + 1, c, v:v + 1]
                if cc % 2 == 0:
                    cx.op("dve", lambda c=c, cc=cc, a_ap=a_ap, s_ap=s_ap: nc.vector.tensor_scalar(
                        out=dst[:, c, dcol:dcol + 128], in0=pa[:, cc * 128:(cc + 1) * 128],
                        scalar1=a_ap, scalar2=s_ap, op0=ALU.mult, op1=ALU.add),
                        r=[pa, self.AS], w=[dst_t])
                else:
                    cx.op("act", lambda c=c, cc=cc, a_ap=a_ap, s_ap=s_ap: nc.scalar.activation(
                        out=dst[:, c, dcol:dcol + 128], in_=pa[:, cc * 128:(cc + 1) * 128],
                        func=AF.Identity, bias=s_ap, scale=a_ap),
                        r=[pa, self.AS], w=[dst_t])
        return x

    def setup_moe(self, n_layers, n_exp):
        self.wgu_d = self.inp("wgu", [n_layers, n_exp, D, 2 * D])
        self.bguT_d = self.inp("bguT", [n_layers, P, n_exp, 16])
        self.wdn_d = self.inp("wdn", [n_layers, n_exp, D, D])
        self.bdn_d = self.inp("bdn", [n_layers, n_exp, D])
        self.wr_d = self.inp("wr", [n_layers, D, n_exp])
        self.br_d = self.inp("br", [n_layers, 1, n_exp])

    def stage_moe(self, l, tiles, res, n_exp, final=None, nsb_tiles=9, sb_sizes=None):
        cx, nc = self.cx, self.nc
        cx.barrier()
        cx.arena_reset()
        TS = nsb_tiles
        if sb_sizes is None:
            sb_sizes = [min(TS, len(tiles) - i) for i in range(0, len(tiles), TS)]
        TS = max(sb_sizes)
        sb_starts = [sum(sb_sizes[:i]) for i in range(len(sb_sizes))]
        hT = cx.sba([P, KC, TS * P], BF16, "hT")
        acc = cx.sba([P, TS, D], F32, "acc")
        acc_t = [T("acc%d" % s) for s in range(TS)]
        aT = cx.sba([P, KC, TS * P], BF16, "aT")
        wt = cx.sba([P, TS, n_exp], F32, "wt")
        bgu = cx.sba([P, n_exp, 16], F32, "bgu")
        bdn = cx.sba([n_exp, D], F32, "bdn")
        wr = cx.sba([P, KC, n_exp], F32, "wr")
        brow = cx.sba([1, n_exp], F32, "brow")
        gring = Ring([cx.sba([P, KC, 256], BF16, "wg%d" % i) for i in range(8)])
        dring = Ring([cx.sba([P, 2, D], BF16, "wd%d" % i) for i in range(5)])
        xr = Ring([cx.sba([P, D], F32, "x%d" % i) for i in range(1)])
        xnr = Ring([cx.sba([P, D], F32, "xn%d" % i) for i in range(1)])
        sst = Ring([cx.sba([P, 4], F32, "ss%d" % i) for i in range(2)])
        tmpr = Ring([cx.sba([P, 512], F32, "tmp%d" % i) for i in range(6)])
        hfr = Ring([cx.sba([P, KC, P], F32, "hf%d" % i) for i in range(1)])
        sm = Ring([cx.sba([P, 4 * n_exp + 16], F32, "sm%d" % i) for i in range(1)])
        wTt = Ring([cx.sba([n_exp, P], F32, "wTt%d" % i) for i in range(2)])
        if final is not None:
            gfB = cx.sba([P, D], F32, "gfB")
            cx.dma("sp", gfB[:], final[0].partition_broadcast(P), w=[gfB])
        cx.dma("sp", bgu[:], self.bguT_d[l], w=[bgu])
        cx.dma("sp", bdn[:], self.bdn_d[l], w=[bdn])
        cx.dma("sp", wr[:], self.wr_d[l].rearrange("(kc p) e -> p kc e", p=P), w=[wr])
        cx.dma("sp", brow[:], self.br_d[l], w=[brow])

        for sb0, sbn in zip(sb_starts, sb_sizes):
            sbt = tiles[sb0:sb0 + sbn]
            n = len(sbt)
            for s, (row0, v) in enumerate(sbt):
                rt = self.dt(("res", row0))
                hf = hfr.next()
                self.norm_mod_T(res[row0:row0 + P, :], rt, 1, v, hf, hf, 0, xr, xnr, sst)
                cx.op("pool", lambda s=s: nc.gpsimd.tensor_copy(hT[:, :, s * P:(s + 1) * P], hf[:]),
                      r=[hf], w=[hT])
                pr = self.pa.next()
                for kc in range(KC):
                    cx.op("pe", lambda kc=kc: nc.tensor.matmul(
                        pr[:, 0:n_exp], hf[:, kc, :], wr[:, kc, :],
                        start=(kc == 0), stop=False), r=[hf, wr], w=[pr])
                cx.op("pe", lambda: nc.tensor.matmul(
                    pr[:, 0:n_exp], self.ones[0:1, :], brow[0:1, :], start=False, stop=True),
                    r=[self.ones, brow], w=[pr])
                m = sm.next()
                lg = m[:, 0:n_exp]
                ex = m[:, n_exp:2 * n_exp]
                mk = m[:, 2 * n_exp:3 * n_exp]
                em = m[:, 3 * n_exp:4 * n_exp]
                t8 = m[:, 4 * n_exp:4 * n_exp + 8]
                sc = m[:, 4 * n_exp + 8:4 * n_exp + 16]
                cx.op("dve", lambda: nc.vector.tensor_copy(lg, pr[:, 0:n_exp]), r=[pr], w=[m])
                cx.op("dve", lambda: nc.vector.max(out=t8, in_=lg), r=[m], w=[m])
                cx.op("dve", lambda: nc.vector.tensor_scalar(
                    out=mk, in0=lg, scalar1=t8[:, 3:4], scalar2=None, op0=ALU.is_ge), r=[m], w=[m])
                cx.op("dve", lambda: nc.vector.tensor_scalar(
                    out=sc[:, 0:1], in0=t8[:, 0:1], scalar1=-1.0, scalar2=None, op0=ALU.mult), r=[m], w=[m])
                cx.op("act", lambda: nc.scalar.activation(out=ex, in_=lg, func=AF.Exp, bias=sc[:, 0:1]),
                      r=[m], w=[m])
                cx.op("dve", lambda: nc.vector.tensor_tensor(out=em, in0=ex, in1=mk, op=ALU.mult),
                      r=[m], w=[m])
                cx.op("dve", lambda: nc.vector.reduce_sum(out=sc[:, 1:2], in_=em, axis=AX.X), r=[m], w=[m])
                cx.op("dve", lambda: nc.vector.reciprocal(out=sc[:, 2:3], in_=sc[:, 1:2]), r=[m], w=[m])
                cx.op("dve", lambda s=s: nc.vector.tensor_scalar(
                    out=wt[:, s, :], in0=em, scalar1=sc[:, 2:3], scalar2=None, op0=ALU.mult),
                    r=[m], w=[wt])
                pw = self.pa.next()
                cx.op("pe", lambda s=s: nc.tensor.transpose(
                    out=pw[0:n_exp, 0:P], in_=wt[:, s, :], identity=self.ident[:]),
                    r=[wt, self.ident], w=[pw])
                wtt = wTt.next()
                cx.op("act", lambda: nc.scalar.copy(out=wtt[:], in_=pw[0:n_exp, 0:P]), r=[pw], w=[wtt])
                py = self.py.next()
                for h in range(2):
                    cx.op("pe", lambda h=h: nc.tensor.matmul(
                        py[:, h * 512:(h + 1) * 512], wtt[:], bdn[:, h * 512:(h + 1) * 512],
                        start=True, stop=True), r=[wtt, bdn], w=[py])
                cx.op("act", lambda s=s: nc.scalar.copy(out=acc[:, s, :], in_=py[:]), r=[py], w=[acc_t[s]])

            blocks = []
            s0 = 0
            while s0 < n:
                nb = min(4, n - s0)
                if n - s0 - nb == 1:
                    nb -= 1
                blocks.append((s0, nb))
                s0 += nb
            aT_t = [T("aT%d" % i) for i in range(len(blocks))]
            for e in range(n_exp):
                wsrc = self.wgu_d[l, e].rearrange("(kc p) n -> p kc n", p=P)
                dsrc = self.wdn_d[l, e].rearrange("(fc p) n -> p fc n", p=P)
                wgs = []
                for fc in range(KC):
                    wch = gring.next()
                    cx.dma("pool", wch[:], wsrc[:, :, fc * 256:(fc + 1) * 256], w=[wch])
                    wgs.append(wch)
                wds = []
                for j in range(4):
                    wd = dring.next()
                    cx.dma("pool", wd[:], dsrc[:, 2 * j:2 * j + 2, :], w=[wd])
                    wds.append(wd)
                for fc in range(KC):
                    wch = wgs[fc]
                    for bi, (bs0, nb) in enumerate(blocks):
                        N = nb * P
                        c0 = bs0 * P
                        pg = self.pa.next()
                        pu = self.pa.next()
                        for kc in range(KC):
                            cx.op("pe", lambda kc=kc: nc.tensor.matmul(
                                pg[:, 0:N], wch[:, kc, 0:128], hT[:, kc, c0:c0 + N],
                                start=(kc == 0), stop=(kc == KC - 1)), r=[wch, hT], w=[pg])
                        for kc in range(KC):
                            cx.op("pe", lambda kc=kc: nc.tensor.matmul(
                                pu[:, 0:N], wch[:, kc, 128:256], hT[:, kc, c0:c0 + N],
                                start=(kc == 0), stop=(kc == KC - 1)), r=[wch, hT], w=[pu])
                        g = tmpr.next()
                        sg = tmpr.next()
                        u = tmpr.next()
                        bg = bgu[:, e, 2 * fc:2 * fc + 1]
                        bu = bgu[:, e, 2 * fc + 1:2 * fc + 2]
                        cx.op("dve", lambda: nc.vector.tensor_scalar(
                            out=g[:, 0:N], in0=pg[:, 0:N], scalar1=bg, scalar2=7.0, op0=ALU.add, op1=ALU.min),
                            r=[pg, bgu], w=[g])
                        cx.op("act", lambda: nc.scalar.activation(
                            out=sg[:, 0:N], in_=g[:, 0:N], func=AF.Sigmoid, scale=1.702), r=[g], w=[sg])
                        cx.op("act", lambda: nc.scalar.activation(
                            out=u[:, 0:N], in_=pu[:, 0:N], func=AF.Identity, bias=bu), r=[pu, bgu], w=[u])
                        cx.op("dve", lambda: nc.vector.tensor_scalar(
                            out=u[:, 0:N], in0=u[:, 0:N], scalar1=-7.0, scalar2=7.0, op0=ALU.max, op1=ALU.min),
                            r=[u], w=[u])
                        cx.op("dve", lambda: nc.vector.tensor_tensor(
                            out=g[:, 0:N], in0=g[:, 0:N], in1=sg[:, 0:N], op=ALU.mult), r=[g, sg], w=[g])
                        cx.op("dve", lambda fc=fc: nc.vector.scalar_tensor_tensor(
                            out=aT[:, fc, c0:c0 + N], in0=u[:, 0:N], scalar=1.0, in1=g[:, 0:N],
                            op0=ALU.add, op1=ALU.mult), r=[u, g], w=[aT_t[bi]])
                for bi, (bs0, nb) in enumerate(blocks):
                    for s in range(bs0, bs0 + nb):
                        py = self.py.next()
                        for h in range(2):
                            for fc in range(KC):
                                cx.op("pe", lambda fc=fc, h=h, s=s: nc.tensor.matmul(
                                    py[:, h * 512:(h + 1) * 512], aT[:, fc, s * P:(s + 1) * P],
                                    wds[fc // 2][:, fc % 2, h * 512:(h + 1) * 512],
                                    start=(fc == 0), stop=(fc == KC - 1)),
                                    r=[aT_t[bi], wds[fc // 2]], w=[py])
                        for h in range(2):
                            cx.op("dve", lambda h=h, s=s: nc.vector.scalar_tensor_tensor(
                                out=acc[:, s, h * 512:(h + 1) * 512], in0=py[:, h * 512:(h + 1) * 512],
                                scalar=wt[:, s, e:e + 1], in1=acc[:, s, h * 512:(h + 1) * 512],
                                op0=ALU.mult, op1=ALU.add), r=[py, wt, acc_t[s]], w=[acc_t[s]])

            for s, (row0, v) in enumerate(sbt):
                rt = self.dt(("res", row0))
                x = xr.next()
                cx.dma("sp", x[:], res[row0:row0 + P, :], r=[rt], w=[x])
                xn = xnr.next()
                cx.op("dve", lambda s=s, v=v: nc.vector.tensor_tensor(
                    out=xn[:], in0=acc[:, s, :], in1=self.gB[v][1][:], op=ALU.mult),
                    r=[acc_t[s], self.gB[v][1]], w=[xn])
                cx.op("dve", lambda: nc.vector.tensor_tensor(out=x[:], in0=x[:], in1=xn[:], op=ALU.add),
                      r=[x, xn], w=[x])
                if final is None:
                    cx.dma("sp", res[row0:row0 + P, :], x[:], r=[x], w=[rt])
                else:
                    ss = sst.next()
                    cx.op("dve", lambda: nc.vector.scalar_tensor_tensor(
                        out=xn[:], in0=x[:], scalar=1.0, in1=x[:], op0=ALU.mult, op1=ALU.mult,
                        accum_out=ss[:, 0:1]), r=[x], w=[xn, ss])
                    self.rstd(ss)
                    cx.op("dve", lambda: nc.vector.scalar_tensor_tensor(
                        out=xn[:], in0=x[:], scalar=ss[:, 2:3], in1=gfB[:], op0=ALU.mult, op1=ALU.mult),
                        r=[x, ss, gfB], w=[xn])
                    orow = row0 - final[2]
                    cx.dma("sp", final[1][orow:orow + P, :], xn[:], r=[xn], w=[self.dt(("out", orow))])

    def stage_outproj(self, tiles, res, oT, w_d):
        cx, nc = self.cx, self.nc
        cx.barrier()
        cx.arena_reset()
        W = cx.sba([P, KC, D], BF16, "Wo")
        cx.dma("pool", W[:], w_d.rearrange("(kc p) n -> p kc n", p=P), w=[W])
        otr = Ring([cx.sba([P, KC, P], BF16, "ot%d" % i) for i in range(3)])
        xr = Ring([cx.sba([P, D], F32, "x%d" % i) for i in range(3)])
        tr = Ring([cx.sba([P, D], F32, "t%d" % i) for i in range(2)])
        osrc = oT.rearrange("(kc p) t -> p kc t", p=P)
        for (row0, v) in tiles:
            rt = self.dt(("res", row0))
            ot = otr.next()
            cx.dma("pool", ot[:], osrc[:, :, row0:row0 + P], r=[self.dt(("oT", row0))], w=[ot])
            x = xr.next()
            cx.dma("sp", x[:], res[row0:row0 + P, :], r=[rt], w=[x])
            py = self.py.next()
            for h in range(2):
                for kc in range(KC):
                    cx.op("pe", lambda kc=kc, h=h: nc.tensor.matmul(
                        py[:, h * 512:(h + 1) * 512], ot[:, kc, :], W[:, kc, h * 512:(h + 1) * 512],
                        start=(kc == 0), stop=(kc == KC - 1)), r=[ot, W], w=[py])
            t = tr.next()
            cx.op("dve", lambda v=v: nc.vector.tensor_tensor(
                out=t[:], in0=py[:], in1=self.gB[v][0][:], op=ALU.mult), r=[py, self.gB[v][0]], w=[t])
            cx.op("pool", lambda: nc.gpsimd.tensor_tensor(out=x[:], in0=x[:], in1=t[:], op=ALU.add),
                  r=[x, t], w=[x])
            cx.dma("sp", res[row0:row0 + P, :], x[:], r=[x], w=[rt])

    def setup_fnet(self):
        self.f_w_in_d = self.inp("f_w_in", [2, D, D])
        self.f_w_out_d = self.inp("f_w_out", [2, D, D])
        self.csc_d = self.inp("csc", [P, 256])
        self.tabL_d = self.inp("tabL", [2, SEQ, SEQ])
        self.tabC_d = self.inp("tabC", [2, CTX, CTX])

    def stage_fnet1(self, j, tiles, res, UC, US):
        cx, nc = self.cx, self.nc
        cx.barrier()
        cx.arena_reset()
        W = cx.sba([P, KC, D], BF16, "Wi")
        cx.dma("pool", W[:], self.f_w_in_d[j].rearrange("(kc p) n -> p kc n", p=P), w=[W])
        csc = cx.sba([P, 256], BF16, "csc")
        cx.dma("pool", csc[:], self.csc_d, w=[csc])
        hTr = Ring([cx.sba([P, KC, 512], BF16, "hT%d" % i) for i in range(2)])
        uTr = Ring([cx.sba([P, KC, 512], BF16, "uT%d" % i) for i in range(2)])
        xr = Ring([cx.sba([P, D], F32, "x%d" % i) for i in range(2)])
        xnr = Ring([cx.sba([P, D], F32, "xn%d" % i) for i in range(2)])
        sst = Ring([cx.sba([P, 4], F32, "ss%d" % i) for i in range(2)])
        ucr = Ring([cx.sba([P, D], F32, "uc%d" % i) for i in range(2)])
        usr = Ring([cx.sba([P, D], F32, "us%d" % i) for i in range(2)])
        for b0 in range(0, len(tiles), 4):
            bt = tiles[b0:b0 + 4]
            N = len(bt) * P
            hT = hTr.next()
            for s, (row0, v) in enumerate(bt):
                self.norm_mod_T(res[row0:row0 + P, :], self.dt(("res", row0)), 0, v, hT, hT, s * P, xr, xnr, sst)
            uT = uTr.next()
            for g in range(KC):
                pa = self.pa.next()
                for kc in range(KC):
                    cx.op("pe", lambda kc=kc, g=g: nc.tensor.matmul(
                        pa[:, 0:N], W[:, kc, g * P:(g + 1) * P], hT[:, kc, 0:N],
                        start=(kc == 0), stop=(kc == KC - 1)), r=[W, hT], w=[pa])
                if g % 2 == 0:
                    cx.op("act", lambda g=g: nc.scalar.copy(out=uT[:, g, 0:N], in_=pa[:, 0:N]), r=[pa], w=[uT])
                else:
                    cx.op("dve", lambda g=g: nc.vector.tensor_copy(uT[:, g, 0:N], pa[:, 0:N]), r=[pa], w=[uT])
            for s, (row0, v) in enumerate(bt):
                uc = ucr.next()
                us = usr.next()
                for gp in range(4):
                    pa = self.pa.next()
                    for gg in range(2):
                        g = 2 * gp + gg
                        cx.op("pe", lambda g=g, gg=gg, s=s: nc.tensor.matmul(
                            pa[:, gg * 256:(gg + 1) * 256], uT[:, g, s * P:(s + 1) * P], csc[:],
                            start=True, stop=True), r=[uT, csc], w=[pa])
                    for gg in range(2):
                        g = 2 * gp + gg
                        cx.op("dve", lambda g=g, gg=gg: nc.vector.tensor_copy(
                            uc[:, g * P:(g + 1) * P], pa[:, gg * 256:gg * 256 + P]), r=[pa], w=[uc])
                        cx.op("dve", lambda g=g, gg=gg: nc.vector.tensor_copy(
                            us[:, g * P:(g + 1) * P], pa[:, gg * 256 + P:(gg + 1) * 256]), r=[pa], w=[us])
                cx.dma("sp", UC[row0:row0 + P, :], uc[:], r=[uc], w=[self.dt(("UC", row0))])
                cx.dma("sp", US[row0:row0 + P, :], us[:], r=[us], w=[self.dt(("US", row0))])

    def stage_fnet2(self, L, row0, tab_d, UC, US, fT):
        cx, nc = self.cx, self.nc
        cx.barrier()
        cx.arena_reset()
        LC = L // P
        LB = min(512, L)
        ucr = Ring([cx.sba([P, LC, 256], BF16, "ucb%d" % i) for i in range(1)])
        usr = Ring([cx.sba([P, LC, 256], BF16, "usb%d" % i) for i in range(1)])
        tcr = Ring([cx.sba([P, LC, LB], BF16, "tc%d" % i) for i in range(2)])
        tsr = Ring([cx.sba([P, LC, LB], BF16, "ts%d" % i) for i in range(2)])
        orr = Ring([cx.sba([P, 512], F32, "o%d" % i) for i in range(3)])
        rows = [self.dt(("UC", row0 + i * P)) for i in range(LC)] + [self.dt(("US", row0 + i * P)) for i in range(LC)]
        for nb in range(4):
            ucb = ucr.next()
            usb = usr.next()
            ucs = UC[row0:row0 + L, nb * 256:(nb + 1) * 256].rearrange("(lc p) n -> p lc n", p=P)
            uss = US[row0:row0 + L, nb * 256:(nb + 1) * 256].rearrange("(lc p) n -> p lc n", p=P)
            for l0 in range(0, LC, 8):
                l1 = min(LC, l0 + 8)
                cx.dma("pool", ucb[:, l0:l1, :], ucs[:, l0:l1, :], r=rows, w=[ucb])
                cx.dma("pool", usb[:, l0:l1, :], uss[:, l0:l1, :], r=rows, w=[usb])
            for lb in range(L // LB):
                tc = tcr.next()
                ts = tsr.next()
                tcs = tab_d[0][:, lb * LB:(lb + 1) * LB].rearrange("(lc p) m -> p lc m", p=P)
                tss = tab_d[1][:, lb * LB:(lb + 1) * LB].rearrange("(lc p) m -> p lc m", p=P)
                for l0 in range(0, LC, 8):
                    l1 = min(LC, l0 + 8)
                    cx.dma("pool", tc[:, l0:l1, :], tcs[:, l0:l1, :], w=[tc])
                    cx.dma("pool", ts[:, l0:l1, :], tss[:, l0:l1, :], w=[ts])
                for n2 in range(2):
                    pa = self.pa.next()
                    for lc in range(LC):
                        cx.op("pe", lambda lc=lc, n2=n2: nc.tensor.matmul(
                            pa[:, 0:LB], ucb[:, lc, n2 * P:(n2 + 1) * P], tc[:, lc, :],
                            start=(lc == 0), stop=False), r=[ucb, tc], w=[pa])
                    for lc in range(LC):
                        cx.op("pe", lambda lc=lc, n2=n2: nc.tensor.matmul(
                            pa[:, 0:LB], usb[:, lc, n2 * P:(n2 + 1) * P], ts[:, lc, :],
                            start=False, stop=(lc == LC - 1)), r=[usb, ts], w=[pa])
                    o = orr.next()
                    if n2 == 0:
                        cx.op("act", lambda: nc.scalar.copy(out=o[:, 0:LB], in_=pa[:, 0:LB]), r=[pa], w=[o])
                    else:
                        cx.op("dve", lambda: nc.vector.tensor_copy(o[:, 0:LB], pa[:, 0:LB]), r=[pa], w=[o])
                    n0 = nb * 256 + n2 * P
                    c0 = row0 + lb * LB
                    wts = [self.dt(("oT", c0 + i * P)) for i in range(LB // P)]
                    cx.dma("sp", fT[n0:n0 + P, c0:c0 + LB], o[:, 0:LB], r=[o], w=wts)

    def run_pipeline(self, tiles, stages):
        n, S = len(tiles), len(stages)
        state = [dict() for _ in tiles]
        for t in range(n + S - 1):
            for si in range(S - 1, -1, -1):
                i = t - si
                if 0 <= i < n:
                    stages[si](tiles[i][0], tiles[i][1], state[i])

    def rope_tok(self, dst, src, nh, hd0, R, ct, tr):
        cx, nc = self.cx, self.nc
        q4 = R // 4
        sv = src[:, :, hd0:hd0 + R].rearrange("p h (a t j) -> p h a t j", a=2, t=2)
        dv = dst[:, :, hd0:hd0 + R].rearrange("p h (a t j) -> p h a t j", a=2, t=2)
        cs = ct[:, 0:2 * q4].rearrange("p (a j) -> p a j", a=2).unsqueeze(1).to_broadcast([P, nh, 2, q4])
        sn = ct[:, 2 * q4:4 * q4].rearrange("p (a j) -> p a j", a=2).unsqueeze(1).to_broadcast([P, nh, 2, q4])
        x1 = sv[:, :, :, 0, :]
        x2 = sv[:, :, :, 1, :]
        n = nh * 2 * q4

        def tv(t):
            return t[:, 0:n].rearrange("p (h a j) -> p h a j", h=nh, a=2)
        t1, t2, t3, t4 = tr.next(), tr.next(), tr.next(), tr.next()
        cx.op("dve", lambda: nc.vector.tensor_tensor(out=tv(t1), in0=x1, in1=cs, op=ALU.mult), r=[self._rs, ct], w=[t1])
        cx.op("pool", lambda: nc.gpsimd.tensor_tensor(out=tv(t2), in0=x2, in1=sn, op=ALU.mult), r=[self._rs, ct], w=[t2])
        cx.op("dve", lambda: nc.vector.tensor_tensor(out=tv(t3), in0=x2, in1=cs, op=ALU.mult), r=[self._rs, ct], w=[t3])
        cx.op("pool", lambda: nc.gpsimd.tensor_tensor(out=tv(t4), in0=x1, in1=sn, op=ALU.mult), r=[self._rs, ct], w=[t4])
        cx.op("dve", lambda: nc.vector.tensor_tensor(out=dv[:, :, :, 0, :], in0=tv(t1), in1=tv(t2), op=ALU.subtract),
              r=[t1, t2], w=[self._rd])
        cx.op("pool", lambda: nc.gpsimd.tensor_tensor(out=dv[:, :, :, 1, :], in0=tv(t3), in1=tv(t4), op=ALU.add),
              r=[t3, t4], w=[self._rd])

    def tok_to_featT(self, src, src_t, chunks, dst, dst_t, col0, stg_ring):
        cx, nc = self.cx, self.nc
        w = chunks[0][1]
        for i0 in range(0, len(chunks), 4):
            grp = chunks[i0:i0 + 4]
            ng = len(grp)
            pa = self.pa.next()
            for k, (c0, _) in enumerate(grp):
                cx.op("pe", lambda k=k, c0=c0: nc.tensor.transpose(
                    out=pa[0:w, k * P:(k + 1) * P], in_=src[:, c0:c0 + w], identity=self.ident[:]),
                    r=[src_t, self.ident], w=[pa])
            stg = stg_ring.next()
            cx.op("dve", lambda ng=ng: nc.vector.tensor_copy(stg[0:w, 0:ng * P], pa[0:w, 0:ng * P]),
                  r=[pa], w=[stg])
            dv = dst[i0 * w:(i0 + ng) * w, col0:col0 + P].rearrange("(c d) t -> d c t", d=w)
            cx.dma("sp", dv, stg[0:w, 0:ng * P].rearrange("d (c t) -> d c t", c=ng), r=[stg], w=[dst_t])

    def stage_attn(self, H, kvmap, dk, QT, KT, V, q0, nq, k0, nk, scale, oT):
        cx, nc = self.cx, self.nc
        cx.barrier()
        cx.arena_reset()
        NKC = nk // P
        kTb = cx.sba([P, nk], BF16, "kT")
        cx.op("pool", lambda: nc.gpsimd.memset(kTb[:], 0.0), w=[kTb])
        Sh = cx.sba([P, P], F32, "Sh")
        cx.op("dve", lambda: nc.vector.memset(Sh[:], 0.0), w=[Sh])
        cx.op("dve", lambda: nc.vector.tensor_copy(Sh[:, 0:64], self.ident[:, 64:128]), r=[self.ident], w=[Sh])
        Vb = cx.sba([P, NKC, P], BF16, "Vb")
        cx.op("dve", lambda: nc.vector.memset(Vb[:], 1.0), w=[Vb])
        qr = Ring([cx.sba([P, 512], BF16, "qT%d" % i) for i in range(2)])
        for qb_ in qr.bufs:
            cx.op("pool", lambda qb_=qb_: nc.gpsimd.memset(qb_[:], 0.0), w=[qb_])
        pr = Ring([cx.sba([P, 512], BF16, "pT%d" % i) for i in range(4)])
        rsr = Ring([cx.sba([P, 512], F32, "rs%d" % i) for i in range(2)])
        for rb_ in rsr.bufs:
            cx.op("dve", lambda rb_=rb_: nc.vector.memset(rb_[:], 0.0), w=[rb_])
        rhr = Ring([cx.sba([64, 512], F32, "rh%d" % i) for i in range(2)])
        orr = Ring([cx.sba([64, 512], F32, "o%d" % i) for i in range(2)])
        Hkv = max(kvmap) + 1
        ktk = [self.dt(("KT", k0 + i * P)) for i in range(NKC)]
        vtk = [self.dt(("V", k0 + i * P)) for i in range(NKC)]
        for kvh in range(Hkv):
            cx.dma("pool", kTb[0:dk, :], KT[kvh * dk:(kvh + 1) * dk, k0:k0 + nk], r=ktk, w=[kTb])
            vsrc = V[k0:k0 + nk, kvh * 64:(kvh + 1) * 64].rearrange("(c p) d -> p c d", p=P)
            for c0 in range(0, NKC, 8):
                c1 = min(NKC, c0 + 8)
                cx.dma("pool", Vb[:, c0:c1, 0:64], vsrc[:, c0:c1, :], r=vtk, w=[Vb])
            for h in [hh for hh in range(H) if kvmap[hh] == kvh]:
                for qb in range(0, nq, 512):
                    N = min(512, nq - qb)
                    qT = qr.next()
                    qtk = [self.dt(("QT", q0 + qb + i * P)) for i in range(N // P)]
                    cx.dma("pool", qT[0:dk, 0:N], QT[h * dk:(h + 1) * dk, q0 + qb:q0 + qb + N], r=qtk, w=[qT])
                    py = self.py.next()
                    pend = []

                    def pv(kc, pT):
                        cx.op("pe", lambda: nc.tensor.matmul(
                            py[:, 0:N], Vb[:, kc, :], pT[:, 0:N], start=(kc == 0), stop=(kc == NKC - 1)),
                            r=[Vb, pT], w=[py])
                    for kc in range(NKC):
                        ps = self.pa.next()
                        cx.op("pe", lambda kc=kc: nc.tensor.matmul(
                            ps[:, 0:N], kTb[:, kc * P:(kc + 1) * P], qT[:, 0:N], start=True, stop=True),
                            r=[kTb, qT], w=[ps])
                        pT = pr.next()
                        cx.op("act", lambda: nc.scalar.activation(
                            out=pT[:, 0:N], in_=ps[:, 0:N], func=AF.Exp, scale=float(scale)), r=[ps], w=[pT])
                        pend.append((kc, pT))
                        if len(pend) > 2:
                            pv(*pend.pop(0))
                    while pend:
                        pv(*pend.pop(0))
                    rs = rsr.next()
                    cx.op("dve", lambda: nc.vector.reciprocal(out=rs[64:128, 0:N], in_=py[64:128, 0:N]),
                          r=[py], w=[rs])
                    cx.op("pe", lambda: nc.tensor.matmul(
                        py[:, 512:512 + N], Sh[:], rs[:, 0:N], start=True, stop=True),
                        r=[Sh, rs], w=[py])
                    rh = rhr.next()
                    cx.op("act", lambda: nc.scalar.activation(
                        out=rh[:, 0:N], in_=py[0:64, 512:512 + N], func=AF.Identity), r=[py], w=[rh])
                    o = orr.next()
                    cx.op("dve", lambda: nc.vector.tensor_tensor(
                        out=o[:, 0:N], in0=py[0:64, 0:N], in1=rh[:, 0:N], op=ALU.mult), r=[py, rh], w=[o])
                    otk = [self.dt(("oT", q0 + qb + i * P)) for i in range(N // P)]
                    cx.dma("sp", oT[h * 64:(h + 1) * 64, q0 + qb:q0 + qb + N], o[:, 0:N], r=[o], w=otk)

    def setup_gqa(self):
        self.gqa_w_qkv_d = self.inp("gqa_w_qkv", [1, D, 1536])
        self.gqa_w_o_d = self.inp("gqa_w_o", [1, D, D])
        self.gqa_g_d = self.inp("gqa_g", [1, 2, 64])
        self.ropeG_d = self.inp("ropeG", [SEQ, 64])

    def stage_gqa_proj(self, j, tiles, res, QT, KT, V):
        cx, nc = self.cx, self.nc
        cx.barrier()
        cx.arena_reset()
        W = cx.sba([P, KC, 1536], BF16, "Wqkv")
        cx.dma("pool", W[:], self.gqa_w_qkv_d[j].rearrange("(kc p) n -> p kc n", p=P), w=[W])
        g2 = cx.sba([P, 2, 64], F32, "g2")
        cx.dma("sp", g2[:].rearrange("p a d -> p (a d)"),
               self.gqa_g_d[j:j + 1].rearrange("o a d -> o (a d)").partition_broadcast(P), w=[g2])
        gqk = cx.sba([P, 20, 64], F32, "gqk")
        cx.op("dve", lambda: nc.vector.tensor_copy(gqk[:, 0:16, :], g2[:, 0:1, :].to_broadcast([P, 16, 64])),
              r=[g2], w=[gqk])
        cx.op("dve", lambda: nc.vector.tensor_copy(gqk[:, 16:20, :], g2[:, 1:2, :].to_broadcast([P, 4, 64])),
              r=[g2], w=[gqk])
        hTr = Ring([cx.sba([P, KC, P], BF16, "hT%d" % i) for i in range(3)])
        xr = Ring([cx.sba([P, D], F32, "x%d" % i) for i in range(2)])
        xnr = Ring([cx.sba([P, D], F32, "xn%d" % i) for i in range(2)])
        sst = Ring([cx.sba([P, 4], F32, "ss%d" % i) for i in range(2)])
        qkr = Ring([cx.sba([P, 1536], F32, "qkv%d" % i) for i in range(3)])
        qnr = Ring([cx.sba([P, 1280], F32, "qn%d" % i) for i in range(4)])
        qrr = Ring([cx.sba([P, 1280], F32, "qr%d" % i) for i in range(3)])
        tr = Ring([cx.sba([P, 640], F32, "tt%d" % i) for i in range(8)])
        s20 = Ring([cx.sba([P, 64], F32, "s20%d" % i) for i in range(2)])
        ctr = Ring([cx.sba([P, 64], F32, "ct%d" % i) for i in range(2)])
        stg = Ring([cx.sba([P, 512], F32, "stg%d" % i) for i in range(8)])
        qrows = [(QT[c * P:(c + 1) * P, :], None) for c in range(8)]
        krows = [(KT[c * P:(c + 1) * P, :], None) for c in range(2)]
        def g0(row0, v, st_):
            hT = hTr.next()
            self.norm_mod_T(res[row0:row0 + P, :], self.dt(("res", row0)), 0, v, hT, hT, 0, xr, xnr, sst)
            st_["hT"] = hT

        def g1(row0, v, st_):
            hT = st_["hT"]
            qkv = qkr.next()
            for b in range(3):
                pa = self.pa.next()
                for kc in range(KC):
                    cx.op("pe", lambda kc=kc, b=b: nc.tensor.matmul(
                        pa[:], hT[:, kc, :], W[:, kc, b * 512:(b + 1) * 512],
                        start=(kc == 0), stop=(kc == KC - 1)), r=[hT, W], w=[pa])
                if b == 1:
                    cx.op("dve", lambda b=b: nc.vector.tensor_copy(qkv[:, b * 512:(b + 1) * 512], pa[:]),
                          r=[pa], w=[qkv])
                else:
                    cx.op("act", lambda b=b: nc.scalar.activation(
                        out=qkv[:, b * 512:(b + 1) * 512], in_=pa[:], func=AF.Identity), r=[pa], w=[qkv])
            cx.dma("sp", V[row0:row0 + P, 0:256], qkv[:, 1280:1536], r=[qkv], w=[self.dt(("V", row0))])
            st_["qkv"] = qkv

        def g2(row0, v, st_):
            qkv = st_["qkv"]
            qn = qnr.next()
            st = s20.next()
            qk3 = qkv[:, 0:1280].rearrange("p (h d) -> p h d", d=64)
            qn3 = qn[:].rearrange("p (h d) -> p h d", d=64)
            cx.op("pool", lambda: nc.gpsimd.tensor_tensor(out=qn3, in0=qk3, in1=qk3, op=ALU.mult), r=[qkv], w=[qn])
            cx.op("dve", lambda: nc.vector.tensor_reduce(out=st[:, 0:20], in_=qn3, axis=AX.X, op=ALU.add),
                  r=[qn], w=[st])
            cx.op("dve", lambda: nc.vector.tensor_scalar(
                out=st[:, 0:20], in0=st[:, 0:20], scalar1=1.0 / 64, scalar2=EPS, op0=ALU.mult, op1=ALU.add),
                r=[st], w=[st])
            cx.op("act", lambda: nc.scalar.sqrt(out=st[:, 0:20], in_=st[:, 0:20]), r=[st], w=[st])
            cx.op("dve", lambda: nc.vector.reciprocal(out=st[:, 32:52], in_=st[:, 0:20]), r=[st], w=[st])
            cx.op("dve", lambda: nc.vector.tensor_tensor(
                out=qn3, in0=qk3, in1=st[:, 32:52].unsqueeze(2).to_broadcast([P, 20, 64]), op=ALU.mult),
                r=[qkv, st], w=[qn])
            cx.op("pool", lambda: nc.gpsimd.tensor_tensor(out=qn3, in0=qn3, in1=gqk[:], op=ALU.mult),
                  r=[qn, gqk], w=[qn])
            st_["qn"] = qn

        def g3(row0, v, st_):
            qn = st_["qn"]
            if v == 0:
                ct = ctr.next()
                cx.dma("sp", ct[:], self.ropeG_d[row0 - CTX:row0 - CTX + P, :], w=[ct])
                qrt = qrr.next()
                self._rs, self._rd = qn, qrt
                self.rope_tok(qrt[:].rearrange("p (h d) -> p h d", d=64),
                              qn[:].rearrange("p (h d) -> p h d", d=64), 20, 0, 64, ct, tr)
                st_["src"] = qrt
            else:
                st_["src"] = qn

        def g4(row0, v, st_):
            src = st_["src"]
            self.tok_to_featT(src, src, [(c * P, P) for c in range(8)], QT[0:1024, :], self.dt(("QT", row0)), row0, stg)
            self.tok_to_featT(src, src, [(c * P, P) for c in range(8, 10)], KT[0:256, :], self.dt(("KT", row0)), row0, stg)

        self.run_pipeline(tiles, [g0, g1, g2, g3, g4])

    def setup_mla(self):
        self.mla_w_in_d = self.inp("mla_w_in", [1, D, 672])
        self.mla_g_qaT_d = self.inp("mla_g_qaT", [1, P, 3])
        self.mla_w_qb_d = self.inp("mla_w_qb", [1, 384, 1536])
        self.mla_g_kvaT_d = self.inp("mla_g_kvaT", [1, P, 2])
        self.mla_w_kvb_d = self.inp("mla_w_kvb", [1, 256, 2048])
        self.mla_w_o_d = self.inp("mla_w_o", [1, D, D])
        self.ropeM_d = self.inp("ropeM", [SEQ, 32])

    def stage_mla_proj(self, j, tiles, res, QT, KT, V):
        cx, nc = self.cx, self.nc
        cx.barrier()
        cx.arena_reset()
        Win = cx.sba([P, KC, 672], BF16, "Win")
        cx.dma("pool", Win[:], self.mla_w_in_d[j].rearrange("(kc p) n -> p kc n", p=P), w=[Win])
        gq = cx.sba([P, 3], F32, "gqa")
        gk = cx.sba([P, 2], F32, "gkva")
        cx.dma("sp", gq[:], self.mla_g_qaT_d[j], w=[gq])
        cx.dma("sp", gk[:], self.mla_g_kvaT_d[j], w=[gk])
        Wq = cx.sba([P, 3, 1536], BF16, "Wq")
        Wk = cx.sba([P, 2, 2048], BF16, "Wk")
        stg_w = cx.sba([P, 2048], F32, "stgw")
        qsrc = self.mla_w_qb_d[j].rearrange("(kc p) n -> p kc n", p=P)
        ksrc = self.mla_w_kvb_d[j].rearrange("(kc p) n -> p kc n", p=P)
        for kc in range(3):
            cx.dma("sp", stg_w[:, 0:1536], qsrc[:, kc, :], w=[stg_w])
            cx.op("dve", lambda kc=kc: nc.vector.tensor_scalar(
                out=Wq[:, kc, :], in0=stg_w[:, 0:1536], scalar1=gq[:, kc:kc + 1], scalar2=None, op0=ALU.mult),
                r=[stg_w, gq], w=[Wq])
        for kc in range(2):
            cx.dma("sp", stg_w[:], ksrc[:, kc, :], w=[stg_w])
            cx.op("dve", lambda kc=kc: nc.vector.tensor_scalar(
                out=Wk[:, kc, :], in0=stg_w[:], scalar1=gk[:, kc:kc + 1], scalar2=None, op0=ALU.mult),
                r=[stg_w, gk], w=[Wk])
        hTr = Ring([cx.sba([P, KC, P], BF16, "hT%d" % i) for i in range(2)])
        xr = Ring([cx.sba([P, D], F32, "x%d" % i) for i in range(2)])
        xnr = Ring([cx.sba([P, D], F32, "xn%d" % i) for i in range(2)])
        sst = Ring([cx.sba([P, 4], F32, "ss%d" % i) for i in range(4)])
        ar = Ring([cx.sba([P, 672], F32, "a%d" % i) for i in range(3)])
        anr = Ring([cx.sba([P, 640], F32, "an%d" % i) for i in range(2)])
        jr = Ring([cx.sba([P, 384], F32, "junk%d" % i) for i in range(1)])
        antr = Ring([cx.sba([P, 5, P], BF16, "anT%d" % i) for i in range(2)])
        qsr = Ring([cx.sba([P, 1536], F32, "q%d" % i) for i in range(3)])
        kvr = Ring([cx.sba([P, 2048], F32, "kv%d" % i) for i in range(3)])
        qfr = Ring([cx.sba([P, 1536], F32, "qf%d" % i) for i in range(2)])
        kfr = Ring([cx.sba([P, 1536], F32, "kf%d" % i) for i in range(2)])
        tr = Ring([cx.sba([P, 256], F32, "tt%d" % i) for i in range(4)])
        kper = Ring([cx.sba([P, 32], F32, "kpe%d" % i) for i in range(2)])
        ctr = Ring([cx.sba([P, 32], F32, "ct%d" % i) for i in range(2)])
        stg = Ring([cx.sba([P, 512], F32, "stg%d" % i) for i in range(8)])
        def stage_a(row0, v):
            hT = hTr.next()
            self.norm_mod_T(res[row0:row0 + P, :], self.dt(("res", row0)), 0, v, hT, hT, 0, xr, xnr, sst)
            a = ar.next()
            p1 = self.pa.next()
            for kc in range(KC):
                cx.op("pe", lambda kc=kc: nc.tensor.matmul(
                    p1[:], hT[:, kc, :], Win[:, kc, 0:512], start=(kc == 0), stop=(kc == KC - 1)),
                    r=[hT, Win], w=[p1])
            p2 = self.pa.next()
            for kc in range(KC):
                cx.op("pe", lambda kc=kc: nc.tensor.matmul(
                    p2[:, 0:160], hT[:, kc, :], Win[:, kc, 512:672], start=(kc == 0), stop=(kc == KC - 1)),
                    r=[hT, Win], w=[p2])
            cx.op("act", lambda: nc.scalar.activation(out=a[:, 0:512], in_=p1[:], func=AF.Identity), r=[p1], w=[a])
            cx.op("dve", lambda: nc.vector.tensor_copy(a[:, 512:672], p2[:, 0:160]), r=[p2], w=[a])
            s1 = sst.next()
            s2 = sst.next()
            junk = jr.next()
            cx.op("dve", lambda: nc.vector.scalar_tensor_tensor(
                out=junk[:, 0:384], in0=a[:, 0:384], scalar=1.0, in1=a[:, 0:384], op0=ALU.mult, op1=ALU.mult,
                accum_out=s1[:, 0:1]), r=[a], w=[junk, s1])
            self.rstd(s1, 384)
            cx.op("dve", lambda: nc.vector.scalar_tensor_tensor(
                out=junk[:, 0:256], in0=a[:, 384:640], scalar=1.0, in1=a[:, 384:640], op0=ALU.mult, op1=ALU.mult,
                accum_out=s2[:, 0:1]), r=[a], w=[junk, s2])
            self.rstd(s2, 256)
            an = anr.next()
            cx.op("act", lambda: nc.scalar.activation(
                out=an[:, 0:384], in_=a[:, 0:384], func=AF.Identity, scale=s1[:, 2:3]), r=[a, s1], w=[an])
            cx.op("dve", lambda: nc.vector.tensor_scalar(
                out=an[:, 384:640], in0=a[:, 384:640], scalar1=s2[:, 2:3], scalar2=None, op0=ALU.mult),
                r=[a, s2], w=[an])
            anT = antr.next()
            pt1 = self.pa.next()
            for c in range(4):
                cx.op("pe", lambda c=c: nc.tensor.transpose(
                    out=pt1[:, c * P:(c + 1) * P], in_=an[:, c * P:(c + 1) * P], identity=self.ident[:]),
                    r=[an, self.ident], w=[pt1])
            pt2 = self.pa.next()
            cx.op("pe", lambda: nc.tensor.transpose(
                out=pt2[:, 0:P], in_=an[:, 4 * P:5 * P], identity=self.ident[:]), r=[an, self.ident], w=[pt2])
            cx.op("act", lambda: nc.scalar.activation(
                out=anT[:, 0:4, :].rearrange("p c t -> p (c t)"), in_=pt1[:], func=AF.Identity), r=[pt1], w=[anT])
            cx.op("dve", lambda: nc.vector.tensor_copy(anT[:, 4, :], pt2[:, 0:P]), r=[pt2], w=[anT])
            q = qsr.next()
            for b in range(3):
                pq = self.pa.next()
                for kc in range(3):
                    cx.op("pe", lambda kc=kc, b=b: nc.tensor.matmul(
                        pq[:], anT[:, kc, :], Wq[:, kc, b * 512:(b + 1) * 512], start=(kc == 0), stop=(kc == 2)),
                        r=[anT, Wq], w=[pq])
                if b == 1:
                    cx.op("dve", lambda b=b: nc.vector.tensor_copy(q[:, b * 512:(b + 1) * 512], pq[:]), r=[pq], w=[q])
                else:
                    cx.op("act", lambda b=b: nc.scalar.activation(
                        out=q[:, b * 512:(b + 1) * 512], in_=pq[:], func=AF.Identity), r=[pq], w=[q])
            kv = kvr.next()
            for b2 in range(2):
                pk = self.py.next()
                for hh in range(2):
                    b = 2 * b2 + hh
                    for kc in range(2):
                        cx.op("pe", lambda kc=kc, b=b, hh=hh: nc.tensor.matmul(
                            pk[:, hh * 512:(hh + 1) * 512], anT[:, 3 + kc, :], Wk[:, kc, b * 512:(b + 1) * 512],
                            start=(kc == 0), stop=(kc == 1)), r=[anT, Wk], w=[pk])
                if b2 == 0:
                    cx.op("dve", lambda b2=b2: nc.vector.tensor_copy(kv[:, b2 * 1024:(b2 + 1) * 1024], pk[:]),
                          r=[pk], w=[kv])
                else:
                    cx.op("act", lambda b2=b2: nc.scalar.activation(
                        out=kv[:, b2 * 1024:(b2 + 1) * 1024], in_=pk[:], func=AF.Identity), r=[pk], w=[kv])
            return a, q, kv

        def stage_b(row0, v, a, q, kv):
            kv3 = kv[:].rearrange("p (h d) -> p h d", d=128)
            q3 = q[:].rearrange("p (h d) -> p h d", d=96)
            cx.dma("sp", V[row0:row0 + P, :].rearrange("p (h d) -> p h d", d=64), kv3[:, :, 64:128],
                   r=[kv], w=[self.dt(("V", row0))])
            kf = kfr.next()
            kf3 = kf[:].rearrange("p (h d) -> p h d", d=96)
            cx.op("pool", lambda: nc.gpsimd.tensor_copy(kf3[:, :, 0:64], kv3[:, :, 0:64]), r=[kv], w=[kf])
            if v == 0:
                ct = ctr.next()
                cx.dma("sp", ct[:], self.ropeM_d[row0 - CTX:row0 - CTX + P, :], w=[ct])
                qf = qfr.next()
                qf3 = qf[:].rearrange("p (h d) -> p h d", d=96)
                cx.op("pool", lambda: nc.gpsimd.tensor_copy(qf3[:, :, 0:64], q3[:, :, 0:64]), r=[q], w=[qf])
                self._rs, self._rd = q, qf
                self.rope_tok(qf3, q3, 16, 64, 32, ct, tr)
                kpe = kper.next()
                self._rs, self._rd = a, kpe
                self.rope_tok(kpe[:].rearrange("p (h d) -> p h d", h=1),
                              a[:, 640:672].rearrange("p (h d) -> p h d", h=1), 1, 0, 32, ct, tr)
                cx.op("dve", lambda: nc.vector.tensor_copy(
                    kf3[:, :, 64:96], kpe[:].unsqueeze(1).to_broadcast([P, 16, 32])), r=[kpe], w=[kf])
                qsrc_t = qf
            else:
                cx.op("dve", lambda: nc.vector.tensor_copy(
                    kf3[:, :, 64:96], a[:, 640:672].unsqueeze(1).to_broadcast([P, 16, 32])), r=[a], w=[kf])
                qsrc_t = q
            self.tok_to_featT(qsrc_t, qsrc_t, [(h * 96, 96) for h in range(16)], QT[0:1536, :],
                              self.dt(("QT", row0)), row0, stg)
            self.tok_to_featT(kf, kf, [(h * 96, 96) for h in range(16)], KT[0:1536, :],
                              self.dt(("KT", row0)), row0, stg)

        pend = None
        for (row0, v) in tiles:
            st_new = stage_a(row0, v)
            if pend is not None:
                stage_b(*pend)
            pend = (row0, v) + st_new
        stage_b(*pend)


def host_common():
    return {"ident": np.eye(P, dtype=np.float32), "ones": np.ones((P, P), np.float32)}


def _featT(v):
    v = np.asarray(v)
    return np.ascontiguousarray(np.swapaxes(v.reshape(v.shape[:-1] + (KC, P)), -1, -2))


def host_mod(c_b, c_ctx, w_mod, b_mod, g_mix, g_ffn):
    cs = np.stack([c_b, c_ctx], axis=-1)
    csT = np.ascontiguousarray(cs.reshape(KC, P, 2).transpose(1, 0, 2))
    b_modT = np.ascontiguousarray(b_mod.reshape(DEPTH, 48, P).transpose(0, 2, 1))
    return {"csT": csT, "w_mod": np.ascontiguousarray(w_mod), "b_modT": b_modT,
            "b_mod": np.ascontiguousarray(b_mod.reshape(DEPTH, 1, 6 * D)),
            "g_mixT": _featT(g_mix), "g_ffnT": _featT(g_ffn)}


def host_moe(w_r, b_r, w_gu, b_gu, w_dn, b_dn):
    nl, ne = w_gu.shape[:2]
    g = w_gu[..., 0::2].reshape(nl, ne, D, KC, P)
    u = w_gu[..., 1::2].reshape(nl, ne, D, KC, P)
    wgu = np.ascontiguousarray(np.stack([g, u], axis=-2).reshape(nl, ne, D, 2 * D))
    bg = b_gu[..., 0::2].reshape(nl, ne, KC, P)
    bu = b_gu[..., 1::2].reshape(nl, ne, KC, P)
    bguT = np.stack([bg, bu], axis=-1)
    bguT = np.ascontiguousarray(bguT.transpose(0, 3, 1, 2, 4).reshape(nl, P, ne, 16))
    return {"wgu": wgu, "bguT": bguT, "wdn": np.ascontiguousarray(w_dn), "bdn": np.ascontiguousarray(b_dn),
            "wr": np.ascontiguousarray(w_r), "br": np.ascontiguousarray(b_r.reshape(nl, 1, ne))}


def host_fnet(f_w_in, f_w_out):
    import ml_dtypes
    k = np.arange(P, dtype=np.float64)
    ang = 2 * np.pi * np.outer(k, k) / P
    csc = np.concatenate([np.cos(ang), np.sin(ang)], axis=1).astype(np.float32)

    def tab(L):
        i = np.arange(L, dtype=np.int64)
        m = (np.outer(i, i) % L).astype(np.float64) * (2 * np.pi / L)
        sc = 1.0 / np.sqrt(L * P)
        return np.stack([np.cos(m) * sc, -np.sin(m) * sc]).astype(np.float32)

    return {"f_w_in": np.ascontiguousarray(f_w_in), "f_w_out": np.ascontiguousarray(f_w_out),
            "csc": csc, "tabL": tab(SEQ), "tabC": tab(CTX)}


def _rope_tab(n_tok, R):
    rows = n_tok // 64
    row = np.repeat(np.arange(rows, dtype=np.float32), 64)
    col = np.tile(np.arange(64, dtype=np.float32), rows)
    nf = R // 4
    inv = (np.float32(10000.0) ** (-np.arange(nf, dtype=np.float32) / nf)).astype(np.float32)
    ang = np.stack([row[:, None] * inv, col[:, None] * inv], axis=1).astype(np.float32)
    return np.concatenate([np.cos(ang).reshape(n_tok, 2 * nf), np.sin(ang).reshape(n_tok, 2 * nf)],
                          axis=1).astype(np.float32)


def host_gqa(w_qkv, g_q, g_k, w_o):
    return {"gqa_w_qkv": np.ascontiguousarray(w_qkv), "gqa_w_o": np.ascontiguousarray(w_o),
            "gqa_g": np.ascontiguousarray(np.stack([g_q, g_k], axis=1)), "ropeG": _rope_tab(SEQ, 64)}


def host_mla(w_in, g_qa, w_qb, g_kva, w_kvb, w_o):
    return {"mla_w_in": np.ascontiguousarray(w_in),
            "mla_g_qaT": np.ascontiguousarray(np.swapaxes(g_qa.reshape(-1, 3, P), 1, 2)),
            "mla_w_qb": np.ascontiguousarray(w_qb),
            "mla_g_kvaT": np.ascontiguousarray(np.swapaxes(g_kva.reshape(-1, 2, P), 1, 2)),
            "mla_w_kvb": np.ascontiguousarray(w_kvb), "mla_w_o": np.ascontiguousarray(w_o),
            "ropeM": _rope_tab(SEQ, 32)}


TT = CTX + SEQ


def build_full():
    p = Prog({})
    cx = p.cx
    p.setup_common()
    p.setup_mod()
    p.setup_fnet()
    p.setup_mla()
    p.setup_gqa()
    p.setup_moe(DEPTH, NE)
    res0_d = p.inp("res0", [TT, D])
    gf_d = p.inp("g_final", [1, D])
    out_d = p.outp("out", [SEQ, D])
    res = p.scratch("res", [TT, D])
    UC = p.scratch("UC", [TT, D])
    US = p.scratch("US", [TT, D])
    oT = p.scratch("oT", [D, TT])
    QT = p.scratch("QT", [1536, TT])
    KT = p.scratch("KT", [1536, TT])
    V = p.scratch("V", [TT, D])
    all_tiles = [(i * P, 1 if i < CTX // P else 0) for i in range(TT // P)]
    lat_tiles = [t for t in all_tiles if t[1] == 0]
    for (r0, v) in all_tiles:
        cx.dma("sp", res[r0:r0 + P, :], res0_d[r0:r0 + P, :], w=[p.dt(("res", r0))])
    p.stage_mod(0)
    p.stage_fnet1(0, all_tiles, res, UC, US)
    p.stage_fnet2(CTX, 0, p.tabC_d, UC, US, oT)
    p.stage_fnet2(SEQ, CTX, p.tabL_d, UC, US, oT)
    p.stage_outproj(all_tiles, res, oT, p.f_w_out_d[0])
    p.stage_moe(0, all_tiles, res, NE, sb_sizes=[10, 8, 8, 8])
    p.stage_mod(1)
    p.stage_mla_proj(0, all_tiles, res, QT, KT, V)
    p.stage_attn(16, list(range(16)), 96, QT, KT, V, CTX, SEQ, 0, TT, 96 ** -0.5, oT)
    p.stage_attn(16, list(range(16)), 96, QT, KT, V, 0, CTX, 0, CTX, 96 ** -0.5, oT)
    p.stage_outproj(all_tiles, res, oT, p.mla_w_o_d[0])
    p.stage_moe(1, all_tiles, res, NE, sb_sizes=[10, 8, 8, 8])
    p.stage_mod(2)
    p.stage_gqa_proj(0, all_tiles, res, QT, KT, V)
    kvmap = [h // 4 for h in range(16)]
    p.stage_attn(16, kvmap, 64, QT, KT, V, CTX, SEQ, 0, TT, 64 ** -0.5, oT)
    p.stage_outproj(lat_tiles, res, oT, p.gqa_w_o_d[0])
    p.stage_moe(2, lat_tiles, res, NE, nsb_tiles=8)
    p.stage_mod(3)
    p.stage_fnet1(1, lat_tiles, res, UC, US)
    p.stage_fnet2(SEQ, CTX, p.tabL_d, UC, US, oT)
    p.stage_outproj(lat_tiles, res, oT, p.f_w_out_d[1])
    p.stage_moe(3, lat_tiles, res, NE, final=(gf_d, out_d, CTX), nsb_tiles=8)
    cx.finish()
    return p


_PROG = None


def kernel(x, c, ctx, c_ctx, w_mod, b_mod, g_mix, g_ffn, g_final, f_w_in, f_w_out,
           mla_w_in, mla_g_qa, mla_w_qb, mla_g_kva, mla_w_kvb, mla_w_o,
           gqa_w_qkv, gqa_g_q, gqa_g_k, gqa_w_o,
           moe_w_router, moe_b_router, moe_w_gu, moe_b_gu, moe_w_down, moe_b_down):
    global _PROG
    f = lambda a: np.asarray(a, dtype=np.float32)
    x, c, ctx, c_ctx = f(x), f(c), f(ctx), f(c_ctx)
    if _PROG is None:
        _PROG = build_full()
    p = _PROG
    shared = host_common()
    shared.update(host_fnet(f(f_w_in), f(f_w_out)))
    shared.update(host_mla(f(mla_w_in), f(mla_g_qa), f(mla_w_qb), f(mla_g_kva), f(mla_w_kvb), f(mla_w_o)))
    shared.update(host_gqa(f(gqa_w_qkv), f(gqa_g_q), f(gqa_g_k), f(gqa_w_o)))
    shared.update(host_moe(f(moe_w_router), f(moe_b_router), f(moe_w_gu), f(moe_b_gu), f(moe_w_down), f(moe_b_down)))
    shared["g_final"] = np.ascontiguousarray(f(g_final).reshape(1, D))
    nb = x.shape[0]
    in_maps = []
    for b in range(nb):
        m = dict(shared)
        m.update(host_mod(c[b], c_ctx, f(w_mod), f(b_mod), f(g_mix), f(g_ffn)))
        m["res0"] = np.ascontiguousarray(np.concatenate([ctx[b], x[b]], axis=0))
        in_maps.append(m)
    r = run_bass_kernel_spmd(p.nc, in_maps, core_ids=list(range(nb)))
    return np.stack([np.asarray(r.results[b]["out"], dtype=np.float32) for b in range(nb)], axis=0)
```

```python
import numpy as np
import concourse.bass as bass
import concourse.mybir as mybir
from concourse.bass_utils import run_bass_kernel_spmd

F32 = mybir.dt.float32
F32R = mybir.dt.float32r
BF16 = mybir.dt.bfloat16
ALU = mybir.AluOpType
AF = mybir.ActivationFunctionType
AX = mybir.AxisListType

P = 128
D = 1024
KC = 8
NE = 32
DEPTH = 4
CTX = 256
SEQ = 4096
EPS = 1e-6
SBUF_BASE = 16512
SBUF_CAP = 229376 - 128


class T:
    __slots__ = ("w", "r", "name")

    def __init__(self, name=""):
        self.w = None
        self.r = {}
        self.name = name


class Buf:
    __slots__ = ("ap", "t")

    def __init__(self, ap, name=""):
        self.ap = ap
        self.t = T(name)

    def __getitem__(self, k):
        return self.ap[k]


class Ring:
    def __init__(self, bufs):
        self.bufs = bufs
        self.i = 0

    def next(self):
        b = self.bufs[self.i % len(self.bufs)]
        self.i += 1
        return b


def _t(b):
    return b.t if isinstance(b, Buf) else b


class Ctx:
    N_DMA_SEMS = {"sp": 10, "pool": 8, "act": 2}

    def __init__(self, nc):
        self.nc = nc
        self.E = {"pe": nc.tensor, "dve": nc.vector, "act": nc.scalar, "pool": nc.gpsimd, "sp": nc.sync}
        self.sem = {}
        self.cnt = {}
        for e in ("pe", "dve", "act", "pool"):
            self.sem[e] = nc.alloc_semaphore("s_" + e)
            self.cnt[e] = 0
        self.dsems = {}
        self.drr = {}
        for q, n in self.N_DMA_SEMS.items():
            self.dsems[q] = []
            self.drr[q] = 0
            for i in range(n):
                nm = "d_%s%d" % (q, i)
                self.sem[nm] = nc.alloc_semaphore(nm)
                self.cnt[nm] = 0
                self.dsems[q].append(nm)
        self.seen = {e: {} for e in self.E}
        self.n_inst = 0
        self._id = 0
        self.pbase = SBUF_BASE
        self.abase = SBUF_BASE

    def _sb_at(self, shape, dtype, off, name):
        self._id += 1
        name = "%s_%d" % (name or "sb", self._id)
        h = self.nc.alloc_sbuf_tensor_at(name, list(shape), dtype, offset=off)
        return Buf(h, name)

    @staticmethod
    def _bytes(shape, dtype):
        n = 1
        for s in shape[1:]:
            n *= s
        n *= 2 if dtype == BF16 else 4
        return (n + 31) // 32 * 32

    def sbp(self, shape, dtype=F32, name=None):
        off = self.pbase
        self.pbase += self._bytes(shape, dtype)
        assert self.pbase <= SBUF_CAP, "persistent sbuf overflow"
        self.abase = max(self.abase, self.pbase)
        return self._sb_at(shape, dtype, off, name)

    def arena_reset(self):
        self.abase = self.pbase

    def sba(self, shape, dtype=F32, name=None):
        off = self.abase
        self.abase += self._bytes(shape, dtype)
        assert self.abase <= SBUF_CAP, "arena sbuf overflow %d" % self.abase
        return self._sb_at(shape, dtype, off, name)

    def ps(self, shape, name=None):
        self._id += 1
        name = "%s_%d" % (name or "ps", self._id)
        h = self.nc.alloc_psum_tensor(name, list(shape), F32)
        return Buf(h, name)

    def _deps(self, r, w):
        deps = {}
        for b in r:
            t = _t(b)
            if t.w is not None:
                s, v = t.w
                if deps.get(s, 0) < v:
                    deps[s] = v
        for b in w:
            t = _t(b)
            if t.w is not None:
                s, v = t.w
                if deps.get(s, 0) < v:
                    deps[s] = v
            for s, v in t.r.items():
                if deps.get(s, 0) < v:
                    deps[s] = v
        return deps

    def _wait(self, eng, deps):
        seen = self.seen[eng]
        e = self.E[eng]
        for s, v in deps.items():
            if s == "pe" and eng == "pe":
                continue
            if seen.get(s, 0) >= v:
                continue
            e.wait_ge(self.sem[s], v)
            seen[s] = v
            self.n_inst += 1

    def _mark(self, r, w, s, v):
        for b in w:
            t = _t(b)
            t.w = (s, v)
            t.r = {}
        for b in r:
            t = _t(b)
            if t.r.get(s, 0) < v:
                t.r[s] = v

    def op(self, eng, fn, r=(), w=()):
        self._wait(eng, self._deps(r, w))
        inst = fn()
        inst.then_inc(self.sem[eng], 1)
        self.cnt[eng] += 1
        self.n_inst += 1
        self._mark(r, w, eng, self.cnt[eng])
        return inst

    def dma(self, q, out, in_, r=(), w=(), **kw):
        lst = self.dsems[q]
        nm = lst[self.drr[q] % len(lst)]
        self.drr[q] += 1
        deps = self._deps(r, w)
        if self.cnt[nm] > 0:
            deps[nm] = max(deps.get(nm, 0), self.cnt[nm])
        self._wait(q, deps)
        inst = self.E[q].dma_start(out=out, in_=in_, **kw)
        self.cnt[nm] += 16
        inst.then_inc(self.sem[nm], 16)
        self.n_inst += 1
        self._mark(r, w, nm, self.cnt[nm])
        return inst

    def barrier(self):
        deps = {s: v for s, v in self.cnt.items() if v > 0}
        for e in self.E:
            self._wait(e, dict(deps))

    def finish(self):
        deps = {s: v for s, v in self.cnt.items() if v > 0}
        self._wait("sp", deps)


class Prog:
    def __init__(self, cfg):
        self.cfg = cfg
        nc = bass.Bass("TRN2", target_bir_lowering=False)
        self.nc = nc
        self.cx = Ctx(nc)
        self.din = {}
        self.dram_t = {}

    def inp(self, name, shape, dtype=F32):
        h = self.nc.dram_tensor(name, list(shape), dtype, kind="ExternalInput")
        self.din[name] = h.ap()
        return h.ap()

    def outp(self, name, shape):
        h = self.nc.dram_tensor(name, list(shape), F32, kind="ExternalOutput")
        return h.ap()

    def scratch(self, name, shape):
        h = self.nc.dram_tensor(name, list(shape), F32, kind="Internal")
        return h.ap()

    def dt(self, key):
        if key not in self.dram_t:
            self.dram_t[key] = T(str(key))
        return self.dram_t[key]

    def setup_common(self):
        cx = self.cx
        self.ident_d = self.inp("ident", [P, P])
        self.ones_d = self.inp("ones", [P, P])
        self.ident = cx.sbp([P, P], F32, "ident")
        self.ones = cx.sbp([P, P], F32, "ones")
        cx.dma("sp", self.ident[:], self.ident_d, w=[self.ident])
        cx.dma("sp", self.ones[:], self.ones_d, w=[self.ones])
        self.pa = Ring([cx.ps([P, 512], "pa%d" % i) for i in range(4)])
        self.py = Ring([cx.ps([P, 1024], "py%d" % i) for i in range(2)])

    def setup_mod(self):
        cx = self.cx
        self.csT_d = self.inp("csT", [P, KC, 2])
        self.w_mod_d = self.inp("w_mod", [DEPTH, D, 6 * D])
        self.b_modT_d = self.inp("b_modT", [DEPTH, P, 48])
        self.b_mod_d = self.inp("b_mod", [DEPTH, 1, 6 * D])
        self.g_mixT_d = self.inp("g_mixT", [DEPTH, P, KC])
        self.g_ffnT_d = self.inp("g_ffnT", [DEPTH, P, KC])
        self.sT = cx.sbp([P, KC, 2], F32, "sT")
        self.sB = [cx.sbp([P, KC, P], F32, "sB%d" % v) for v in range(2)]
        self.modT = cx.sbp([P, 48, 2], F32, "modT")
        self.AS = cx.sbp([P, 4, KC, 2], F32, "AS")
        self.gB = [[cx.sbp([P, D], F32, "gB%d%d" % (v, k)) for k in range(2)] for v in range(2)]
        self.bmT = cx.sbp([P, 48], F32, "bmT")
        self.gT = cx.sbp([P, 2, KC], F32, "gT")
        self.bmrow = cx.sbp([1, 2 * D], F32, "bmrow")
        cs = cx.sbp([P, KC, 2], F32, "cs")
        cx.dma("sp", cs[:], self.csT_d, w=[cs])
        cx.op("act", lambda: self.nc.scalar.activation(out=self.sT[:], in_=cs[:], func=AF.Silu),
              r=[cs], w=[self.sT])
        for v in range(2):
            cx.op("dve", lambda v=v: self.nc.vector.tensor_copy(
                self.sB[v][:], self.sT[:, :, v:v + 1].to_broadcast([P, KC, P])),
                r=[self.sT], w=[self.sB[v]])

    def stage_mod(self, l):
        cx, nc = self.cx, self.nc
        cx.barrier()
        cx.arena_reset()
        wring = Ring([cx.sba([P, KC, 256], F32, "wm%d" % i) for i in range(4)])
        cx.dma("sp", self.bmT[:], self.b_modT_d[l], w=[self.bmT])
        cx.dma("sp", self.bmrow[:, 0:D], self.b_mod_d[l][:, 2 * D:3 * D], w=[self.bmrow])
        cx.dma("sp", self.bmrow[:, D:2 * D], self.b_mod_d[l][:, 5 * D:6 * D], w=[self.bmrow])
        cx.dma("sp", self.gT[:, 0, :], self.g_mixT_d[l], w=[self.gT])
        cx.dma("sp", self.gT[:, 1, :], self.g_ffnT_d[l], w=[self.gT])
        wsrc = self.w_mod_d[l].rearrange("(kc p) n -> p kc n", p=P)
        for c in range(24):
            c0 = c * 256
            wch = wring.next()
            cx.dma("sp", wch[:], wsrc[:, :, c0:c0 + 256], w=[wch])
            pa = self.pa.next()
            for jj in range(2):
                j = 2 * c + jj
                for kc in range(KC):
                    cx.op("pe", lambda kc=kc, jj=jj: nc.tensor.matmul(
                        pa[:, jj * 2:jj * 2 + 2], wch[:, kc, jj * 128:(jj + 1) * 128], self.sT[:, kc, :],
                        start=(kc == 0), stop=(kc == KC - 1)), r=[wch, self.sT], w=[pa])
                cx.op("dve", lambda j=j, jj=jj: nc.vector.tensor_scalar(
                    out=self.modT[:, j, :], in0=pa[:, jj * 2:jj * 2 + 2], scalar1=self.bmT[:, j:j + 1],
                    scalar2=None, op0=ALU.add), r=[pa, self.bmT], w=[self.modT])
            m = c0 // D
            if m in (2, 5):
                k = 0 if m == 2 else 1
                cc0 = c0 - m * D
                for v in range(2):
                    pb = self.pa.next()
                    for kc in range(KC):
                        cx.op("pe", lambda kc=kc, v=v: nc.tensor.matmul(
                            pb[:, 0:256], self.sB[v][:, kc, :], wch[:, kc, :],
                            start=(kc == 0), stop=False), r=[wch, self.sB[v]], w=[pb])
                    cx.op("pe", lambda k=k, cc0=cc0: nc.tensor.matmul(
                        pb[:, 0:256], self.ones[0:1, :], self.bmrow[0:1, k * D + cc0:k * D + cc0 + 256],
                        start=False, stop=True), r=[self.ones, self.bmrow], w=[pb])
                    cx.op("act", lambda v=v, k=k, cc0=cc0: nc.scalar.copy(
                        out=self.gB[v][k][:, cc0:cc0 + 256], in_=pb[:, 0:256]), r=[pb], w=[self.gB[v][k]])
        for which, (mshift, mscale) in enumerate(((0, 1), (3, 4))):
            for v in range(2):
                cx.op("dve", lambda which=which, mscale=mscale, v=v: nc.vector.scalar_tensor_tensor(
                    out=self.AS[:, 2 * which, :, v], in0=self.modT[:, mscale * 8:mscale * 8 + 8, v],
                    scalar=1.0, in1=self.gT[:, which, :], op0=ALU.add, op1=ALU.mult),
                    r=[self.modT, self.gT], w=[self.AS])
                cx.op("dve", lambda which=which, mshift=mshift, v=v: nc.vector.tensor_copy(
                    self.AS[:, 2 * which + 1, :, v], self.modT[:, mshift * 8:mshift * 8 + 8, v]),
                    r=[self.modT], w=[self.AS])

    def rstd(self, ss, n=D):
        cx, nc = self.cx, self.nc
        cx.op("dve", lambda: nc.vector.tensor_scalar(
            out=ss[:, 1:2], in0=ss[:, 0:1], scalar1=1.0 / n, scalar2=EPS, op0=ALU.mult, op1=ALU.add),
            r=[ss], w=[ss])
        cx.op("act", lambda: nc.scalar.sqrt(out=ss[:, 1:2], in_=ss[:, 1:2]), r=[ss], w=[ss])
        cx.op("dve", lambda: nc.vector.reciprocal(out=ss[:, 2:3], in_=ss[:, 1:2]), r=[ss], w=[ss])

    def norm_mod_T(self, src_ap, src_t, which, v, dst, dst_t, dcol, xr, xnr, sst):
        cx, nc = self.cx, self.nc
        x = xr.next()
        cx.dma("sp", x[:], src_ap, r=[src_t], w=[x])
        xn = xnr.next()
        ss = sst.next()
        cx.op("dve", lambda: nc.vector.scalar_tensor_tensor(
            out=xn[:], in0=x[:], scalar=1.0, in1=x[:], op0=ALU.mult, op1=ALU.mult, accum_out=ss[:, 0:1]),
            r=[x], w=[xn, ss])
        self.rstd(ss)
        cx.op("act", lambda: nc.scalar.activation(out=xn[:], in_=x[:], func=AF.Identity, scale=ss[:, 2:3]),
              r=[x, ss], w=[xn])
        for hb in range(2):
            pa = self.pa.next()
            for cc in range(4):
                c = hb * 4 + cc
                cx.op("pe", lambda c=c, cc=cc: nc.tensor.transpose(
                    out=pa[:, cc * 128:(cc + 1) * 128], in_=xn[:, c * 128:(c + 1) * 128], identity=self.ident[:]),
                    r=[xn, self.ident], w=[pa])
            for cc in range(4):
                c = hb * 4 + cc
                a_ap = self.AS[:, 2 * which, c, v:v + 1]
                s_ap = self.AS[:, 2 * which + 1, c, v:v + 1]
                if cc % 2 == 0:
                    cx.op("dve", lambda c=c, cc=cc, a_ap=a_ap, s_ap=s_ap: nc.vector.tensor_scalar(
                        out=dst[:, c, dcol:dcol + 128], in0=pa[:, cc * 128:(cc + 1) * 128],
                        scalar1=a_ap, scalar2=s_ap, op0=ALU.mult, op1=ALU.add),
                        r=[pa, self.AS], w=[dst_t])
                else:
                    cx.op("act", lambda c=c, cc=cc, a_ap=a_ap, s_ap=s_ap: nc.scalar.activation(
                        out=dst[:, c, dcol:dcol + 128], in_=pa[:, cc * 128:(cc + 1) * 128],
                        func=AF.Identity, bias=s_ap, scale=a_ap),
                        r=[pa, self.AS], w=[dst_t])
        return x

    def setup_moe(self, n_layers, n_exp):
        self.wgu_d = self.inp("wgu", [n_layers, n_exp, D, 2 * D])
        self.bguT_d = self.inp("bguT", [n_layers, P, n_exp, 16])
        self.wdn_d = self.inp("wdn", [n_layers, n_exp, D, D])
        self.bdn_d = self.inp("bdn", [n_layers, n_exp, D])
        self.wr_d = self.inp("wr", [n_layers, D, n_exp])
        self.br_d = self.inp("br", [n_layers, 1, n_exp])

    def stage_moe(self, l, tiles, res, n_exp, final=None, nsb_tiles=9, sb_sizes=None):
        cx, nc = self.cx, self.nc
        cx.barrier()
        cx.arena_reset()
        TS = nsb_tiles
        if sb_sizes is None:
            sb_sizes = [min(TS, len(tiles) - i) for i in range(0, len(tiles), TS)]
        TS = max(sb_sizes)
        sb_starts = [sum(sb_sizes[:i]) for i in range(len(sb_sizes))]
        hT = cx.sba([P, KC, TS * P], BF16, "hT")
        acc = cx.sba([P, TS, D], F32, "acc")
        acc_t = [T("acc%d" % s) for s in range(TS)]
        aT = cx.sba([P, KC, TS * P], BF16, "aT")
        wt = cx.sba([P, TS, n_exp], F32, "wt")
        bgu = cx.sba([P, n_exp, 16], F32, "bgu")
        bdn = cx.sba([n_exp, D], F32, "bdn")
        wr = cx.sba([P, KC, n_exp], F32, "wr")
        brow = cx.sba([1, n_exp], F32, "brow")
        gring = Ring([cx.sba([P, KC, 256], BF16, "wg%d" % i) for i in range(8)])
        dring = Ring([cx.sba([P, 2, D], BF16, "wd%d" % i) for i in range(5)])
        xr = Ring([cx.sba([P, D], F32, "x%d" % i) for i in range(2)])
        xnr = Ring([cx.sba([P, D], F32, "xn%d" % i) for i in range(1)])
        sst = Ring([cx.sba([P, 4], F32, "ss%d" % i) for i in range(2)])
        tmpr = Ring([cx.sba([P, 512], F32, "tmp%d" % i) for i in range(6)])
        hfr = Ring([cx.sba([P, KC, P], F32, "hf%d" % i) for i in range(1)])
        sm = Ring([cx.sba([P, 4 * n_exp + 16], F32, "sm%d" % i) for i in range(2)])
        wTt = Ring([cx.sba([n_exp, P], F32, "wTt%d" % i) for i in range(2)])
        if final is not None:
            gfB = cx.sba([P, D], F32, "gfB")
            cx.dma("sp", gfB[:], final[0].partition_broadcast(P), w=[gfB])
        cx.dma("sp", bgu[:], self.bguT_d[l], w=[bgu])
        cx.dma("sp", bdn[:], self.bdn_d[l], w=[bdn])
        cx.dma("sp", wr[:], self.wr_d[l].rearrange("(kc p) e -> p kc e", p=P), w=[wr])
        cx.dma("sp", brow[:], self.br_d[l], w=[brow])

        for sb0, sbn in zip(sb_starts, sb_sizes):
            sbt = tiles[sb0:sb0 + sbn]
            n = len(sbt)
            for s, (row0, v) in enumerate(sbt):
                rt = self.dt(("res", row0))
                hf = hfr.next()
                self.norm_mod_T(res[row0:row0 + P, :], rt, 1, v, hf, hf, 0, xr, xnr, sst)
                cx.op("pool", lambda s=s: nc.gpsimd.tensor_copy(hT[:, :, s * P:(s + 1) * P], hf[:]),
                      r=[hf], w=[hT])
                pr = self.pa.next()
                for kc in range(KC):
                    cx.op("pe", lambda kc=kc: nc.tensor.matmul(
                        pr[:, 0:n_exp], hf[:, kc, :], wr[:, kc, :],
                        start=(kc == 0), stop=False), r=[hf, wr], w=[pr])
                cx.op("pe", lambda: nc.tensor.matmul(
                    pr[:, 0:n_exp], self.ones[0:1, :], brow[0:1, :], start=False, stop=True),
                    r=[self.ones, brow], w=[pr])
                m = sm.next()
                lg = m[:, 0:n_exp]
                ex = m[:, n_exp:2 * n_exp]
                mk = m[:, 2 * n_exp:3 * n_exp]
                em = m[:, 3 * n_exp:4 * n_exp]
                t8 = m[:, 4 * n_exp:4 * n_exp + 8]
                sc = m[:, 4 * n_exp + 8:4 * n_exp + 16]
                cx.op("dve", lambda: nc.vector.tensor_copy(lg, pr[:, 0:n_exp]), r=[pr], w=[m])
                cx.op("dve", lambda: nc.vector.max(out=t8, in_=lg), r=[m], w=[m])
                cx.op("dve", lambda: nc.vector.tensor_scalar(
                    out=mk, in0=lg, scalar1=t8[:, 3:4], scalar2=None, op0=ALU.is_ge), r=[m], w=[m])
                cx.op("dve", lambda: nc.vector.tensor_scalar(
                    out=sc[:, 0:1], in0=t8[:, 0:1], scalar1=-1.0, scalar2=None, op0=ALU.mult), r=[m], w=[m])
                cx.op("act", lambda: nc.scalar.activation(out=ex, in_=lg, func=AF.Exp, bias=sc[:, 0:1]),
                      r=[m], w=[m])
                cx.op("dve", lambda: nc.vector.tensor_tensor(out=em, in0=ex, in1=mk, op=ALU.mult),
                      r=[m], w=[m])
                cx.op("dve", lambda: nc.vector.reduce_sum(out=sc[:, 1:2], in_=em, axis=AX.X), r=[m], w=[m])
                cx.op("dve", lambda: nc.vector.reciprocal(out=sc[:, 2:3], in_=sc[:, 1:2]), r=[m], w=[m])
                cx.op("dve", lambda s=s: nc.vector.tensor_scalar(
                    out=wt[:, s, :], in0=em, scalar1=sc[:, 2:3], scalar2=None, op0=ALU.mult),
                    r=[m], w=[wt])
                pw = self.pa.next()
                cx.op("pe", lambda s=s: nc.tensor.transpose(
                    out=pw[0:n_exp, 0:P], in_=wt[:, s, :], identity=self.ident[:]),
                    r=[wt, self.ident], w=[pw])
                wtt = wTt.next()
                cx.op("act", lambda: nc.scalar.copy(out=wtt[:], in_=pw[0:n_exp, 0:P]), r=[pw], w=[wtt])
                py = self.py.next()
                for h in range(2):
                    cx.op("pe", lambda h=h: nc.tensor.matmul(
                        py[:, h * 512:(h + 1) * 512], wtt[:], bdn[:, h * 512:(h + 1) * 512],
                        start=True, stop=True), r=[wtt, bdn], w=[py])
                cx.op("act", lambda s=s: nc.scalar.copy(out=acc[:, s, :], in_=py[:]), r=[py], w=[acc_t[s]])

            blocks = []
            s0 = 0
            while s0 < n:
                nb = min(4, n - s0)
                if n - s0 - nb == 1:
                    nb -= 1
                blocks.append((s0, nb))
                s0 += nb
            aT_t = [T("aT%d" % i) for i in range(len(blocks))]
            for e in range(n_exp):
                wsrc = self.wgu_d[l, e].rearrange("(kc p) n -> p kc n", p=P)
                dsrc = self.wdn_d[l, e].rearrange("(fc p) n -> p fc n", p=P)
                wgs = []
                for fc in range(KC):
                    wch = gring.next()
                    cx.dma("pool", wch[:], wsrc[:, :, fc * 256:(fc + 1) * 256], w=[wch])
                    wgs.append(wch)
                wds = []
                for j in range(4):
                    wd = dring.next()
                    cx.dma("pool", wd[:], dsrc[:, 2 * j:2 * j + 2, :], w=[wd])
                    wds.append(wd)
                for fc in range(KC):
                    wch = wgs[fc]
                    for bi, (bs0, nb) in enumerate(blocks):
                        N = nb * P
                        c0 = bs0 * P
                        pg = self.pa.next()
                        pu = self.pa.next()
                        for kc in range(KC):
                            cx.op("pe", lambda kc=kc: nc.tensor.matmul(
                                pg[:, 0:N], wch[:, kc, 0:128], hT[:, kc, c0:c0 + N],
                                start=(kc == 0), stop=(kc == KC - 1)), r=[wch, hT], w=[pg])
                        for kc in range(KC):
                            cx.op("pe", lambda kc=kc: nc.tensor.matmul(
                                pu[:, 0:N], wch[:, kc, 128:256], hT[:, kc, c0:c0 + N],
                                start=(kc == 0), stop=(kc == KC - 1)), r=[wch, hT], w=[pu])
                        g = tmpr.next()
                        sg = tmpr.next()
                        u = tmpr.next()
                        bg = bgu[:, e, 2 * fc:2 * fc + 1]
                        bu = bgu[:, e, 2 * fc + 1:2 * fc + 2]
                        cx.op("dve", lambda: nc.vector.tensor_scalar(
                            out=g[:, 0:N], in0=pg[:, 0:N], scalar1=bg, scalar2=7.0, op0=ALU.add, op1=ALU.min),
                            r=[pg, bgu], w=[g])
                        cx.op("act", lambda: nc.scalar.activation(
                            out=sg[:, 0:N], in_=g[:, 0:N], func=AF.Sigmoid, scale=1.702), r=[g], w=[sg])
                        cx.op("act", lambda: nc.scalar.activation(
                            out=u[:, 0:N], in_=pu[:, 0:N], func=AF.Identity, bias=bu), r=[pu, bgu], w=[u])
                        cx.op("dve", lambda: nc.vector.tensor_scalar(
                            out=u[:, 0:N], in0=u[:, 0:N], scalar1=-7.0, scalar2=7.0, op0=ALU.max, op1=ALU.min),
                            r=[u], w=[u])
                        cx.op("dve", lambda: nc.vector.tensor_tensor(
                            out=g[:, 0:N], in0=g[:, 0:N], in1=sg[:, 0:N], op=ALU.mult), r=[g, sg], w=[g])
                        cx.op("dve", lambda fc=fc: nc.vector.scalar_tensor_tensor(
                            out=aT[:, fc, c0:c0 + N], in0=u[:, 0:N], scalar=1.0, in1=g[:, 0:N],
                            op0=ALU.add, op1=ALU.mult), r=[u, g], w=[aT_t[bi]])
                for bi, (bs0, nb) in enumerate(blocks):
                    for s in range(bs0, bs0 + nb):
                        py = self.py.next()
                        for h in range(2):
                            for fc in range(KC):
                                cx.op("pe", lambda fc=fc, h=h, s=s: nc.tensor.matmul(
                                    py[:, h * 512:(h + 1) * 512], aT[:, fc, s * P:(s + 1) * P],
                                    wds[fc // 2][:, fc % 2, h * 512:(h + 1) * 512],
                                    start=(fc == 0), stop=(fc == KC - 1)),
                                    r=[aT_t[bi], wds[fc // 2]], w=[py])
                        for h in range(2):
                            cx.op("dve", lambda h=h, s=s: nc.vector.scalar_tensor_tensor(
                                out=acc[:, s, h * 512:(h + 1) * 512], in0=py[:, h * 512:(h + 1) * 512],
                                scalar=wt[:, s, e:e + 1], in1=acc[:, s, h * 512:(h + 1) * 512],
                                op0=ALU.mult, op1=ALU.add), r=[py, wt, acc_t[s]], w=[acc_t[s]])

            for s, (row0, v) in enumerate(sbt):
                rt = self.dt(("res", row0))
                x = xr.next()
                cx.dma("sp", x[:], res[row0:row0 + P, :], r=[rt], w=[x])
                xn = xnr.next()
                cx.op("dve", lambda s=s, v=v: nc.vector.tensor_tensor(
                    out=xn[:], in0=acc[:, s, :], in1=self.gB[v][1][:], op=ALU.mult),
                    r=[acc_t[s], self.gB[v][1]], w=[xn])
                cx.op("dve", lambda: nc.vector.tensor_tensor(out=x[:], in0=x[:], in1=xn[:], op=ALU.add),
                      r=[x, xn], w=[x])
                if final is None:
                    cx.dma("sp", res[row0:row0 + P, :], x[:], r=[x], w=[rt])
                else:
                    ss = sst.next()
                    cx.op("dve", lambda: nc.vector.scalar_tensor_tensor(
                        out=xn[:], in0=x[:], scalar=1.0, in1=x[:], op0=ALU.mult, op1=ALU.mult,
                        accum_out=ss[:, 0:1]), r=[x], w=[xn, ss])
                    self.rstd(ss)
                    cx.op("dve", lambda: nc.vector.scalar_tensor_tensor(
                        out=xn[:], in0=x[:], scalar=ss[:, 2:3], in1=gfB[:], op0=ALU.mult, op1=ALU.mult),
                        r=[x, ss, gfB], w=[xn])
                    orow = row0 - final[2]
                    cx.dma("sp", final[1][orow:orow + P, :], xn[:], r=[xn], w=[self.dt(("out", orow))])

    def stage_outproj(self, tiles, res, oT, w_d):
        cx, nc = self.cx, self.nc
        cx.barrier()
        cx.arena_reset()
        W = cx.sba([P, KC, D], BF16, "Wo")
        cx.dma("pool", W[:], w_d.rearrange("(kc p) n -> p kc n", p=P), w=[W])
        otr = Ring([cx.sba([P, KC, P], BF16, "ot%d" % i) for i in range(3)])
        xr = Ring([cx.sba([P, D], F32, "x%d" % i) for i in range(3)])
        tr = Ring([cx.sba([P, D], F32, "t%d" % i) for i in range(2)])
        osrc = oT.rearrange("(kc p) t -> p kc t", p=P)
        for (row0, v) in tiles:
            rt = self.dt(("res", row0))
            ot = otr.next()
            cx.dma("pool", ot[:], osrc[:, :, row0:row0 + P], r=[self.dt(("oT", row0))], w=[ot])
            x = xr.next()
            cx.dma("sp", x[:], res[row0:row0 + P, :], r=[rt], w=[x])
            py = self.py.next()
            for h in range(2):
                for kc in range(KC):
                    cx.op("pe", lambda kc=kc, h=h: nc.tensor.matmul(
                        py[:, h * 512:(h + 1) * 512], ot[:, kc, :], W[:, kc, h * 512:(h + 1) * 512],
                        start=(kc == 0), stop=(kc == KC - 1)), r=[ot, W], w=[py])
            t = tr.next()
            cx.op("dve", lambda v=v: nc.vector.tensor_tensor(
                out=t[:], in0=py[:], in1=self.gB[v][0][:], op=ALU.mult), r=[py, self.gB[v][0]], w=[t])
            cx.op("pool", lambda: nc.gpsimd.tensor_tensor(out=x[:], in0=x[:], in1=t[:], op=ALU.add),
                  r=[x, t], w=[x])
            cx.dma("sp", res[row0:row0 + P, :], x[:], r=[x], w=[rt])

    def setup_fnet(self):
        self.f_w_in_d = self.inp("f_w_in", [2, D, D])
        self.f_w_out_d = self.inp("f_w_out", [2, D, D])
        self.csc_d = self.inp("csc", [P, 256])
        self.tabL_d = self.inp("tabL", [2, SEQ, SEQ])
        self.tabC_d = self.inp("tabC", [2, CTX, CTX])

    def stage_fnet1(self, j, tiles, res, UC, US):
        cx, nc = self.cx, self.nc
        cx.barrier()
        cx.arena_reset()
        W = cx.sba([P, KC, D], BF16, "Wi")
        cx.dma("pool", W[:], self.f_w_in_d[j].rearrange("(kc p) n -> p kc n", p=P), w=[W])
        csc = cx.sba([P, 256], BF16, "csc")
        cx.dma("pool", csc[:], self.csc_d, w=[csc])
        hTr = Ring([cx.sba([P, KC, 512], BF16, "hT%d" % i) for i in range(2)])
        uTr = Ring([cx.sba([P, KC, 512], BF16, "uT%d" % i) for i in range(2)])
        xr = Ring([cx.sba([P, D], F32, "x%d" % i) for i in range(2)])
        xnr = Ring([cx.sba([P, D], F32, "xn%d" % i) for i in range(2)])
        sst = Ring([cx.sba([P, 4], F32, "ss%d" % i) for i in range(2)])
        ucr = Ring([cx.sba([P, D], F32, "uc%d" % i) for i in range(2)])
        usr = Ring([cx.sba([P, D], F32, "us%d" % i) for i in range(2)])
        for b0 in range(0, len(tiles), 4):
            bt = tiles[b0:b0 + 4]
            N = len(bt) * P
            hT = hTr.next()
            for s, (row0, v) in enumerate(bt):
                self.norm_mod_T(res[row0:row0 + P, :], self.dt(("res", row0)), 0, v, hT, hT, s * P, xr, xnr, sst)
            uT = uTr.next()
            for g in range(KC):
                pa = self.pa.next()
                for kc in range(KC):
                    cx.op("pe", lambda kc=kc, g=g: nc.tensor.matmul(
                        pa[:, 0:N], W[:, kc, g * P:(g + 1) * P], hT[:, kc, 0:N],
                        start=(kc == 0), stop=(kc == KC - 1)), r=[W, hT], w=[pa])
                if g % 2 == 0:
                    cx.op("act", lambda g=g: nc.scalar.copy(out=uT[:, g, 0:N], in_=pa[:, 0:N]), r=[pa], w=[uT])
                else:
                    cx.op("dve", lambda g=g: nc.vector.tensor_copy(uT[:, g, 0:N], pa[:, 0:N]), r=[pa], w=[uT])
            for s, (row0, v) in enumerate(bt):
                uc = ucr.next()
                us = usr.next()
                for gp in range(4):
                    pa = self.pa.next()
                    for gg in range(2):
                        g = 2 * gp + gg
                        cx.op("pe", lambda g=g, gg=gg, s=s: nc.tensor.matmul(
                            pa[:, gg * 256:(gg + 1) * 256], uT[:, g, s * P:(s + 1) * P], csc[:],
                            start=True, stop=True), r=[uT, csc], w=[pa])
                    for gg in range(2):
                        g = 2 * gp + gg
                        cx.op("dve", lambda g=g, gg=gg: nc.vector.tensor_copy(
                            uc[:, g * P:(g + 1) * P], pa[:, gg * 256:gg * 256 + P]), r=[pa], w=[uc])
                        cx.op("dve", lambda g=g, gg=gg: nc.vector.tensor_copy(
                            us[:, g * P:(g + 1) * P], pa[:, gg * 256 + P:(gg + 1) * 256]), r=[pa], w=[us])
                cx.dma("sp", UC[row0:row0 + P, :], uc[:], r=[uc], w=[self.dt(("UC", row0))])
                cx.dma("sp", US[row0:row0 + P, :], us[:], r=[us], w=[self.dt(("US", row0))])

    def stage_fnet2(self, L, row0, tab_d, UC, US, fT):
        cx, nc = self.cx, self.nc
        cx.barrier()
        cx.arena_reset()
        LC = L // P
        LB = min(512, L)
        ucr = Ring([cx.sba([P, LC, 256], BF16, "ucb%d" % i) for i in range(1)])
        usr = Ring([cx.sba([P, LC, 256], BF16, "usb%d" % i) for i in range(1)])
        tcr = Ring([cx.sba([P, LC, LB], BF16, "tc%d" % i) for i in range(2)])
        tsr = Ring([cx.sba([P, LC, LB], BF16, "ts%d" % i) for i in range(2)])
        orr = Ring([cx.sba([P, 512], F32, "o%d" % i) for i in range(3)])
        rows = [self.dt(("UC", row0 + i * P)) for i in range(LC)] + [self.dt(("US", row0 + i * P)) for i in range(LC)]
        for nb in range(4):
            ucb = ucr.next()
            usb = usr.next()
            ucs = UC[row0:row0 + L, nb * 256:(nb + 1) * 256].rearrange("(lc p) n -> p lc n", p=P)
            uss = US[row0:row0 + L, nb * 256:(nb + 1) * 256].rearrange("(lc p) n -> p lc n", p=P)
            for l0 in range(0, LC, 8):
                l1 = min(LC, l0 + 8)
                cx.dma("pool", ucb[:, l0:l1, :], ucs[:, l0:l1, :], r=rows, w=[ucb])
                cx.dma("pool", usb[:, l0:l1, :], uss[:, l0:l1, :], r=rows, w=[usb])
            for lb in range(L // LB):
                tc = tcr.next()
                ts = tsr.next()
                tcs = tab_d[0][:, lb * LB:(lb + 1) * LB].rearrange("(lc p) m -> p lc m", p=P)
                tss = tab_d[1][:, lb * LB:(lb + 1) * LB].rearrange("(lc p) m -> p lc m", p=P)
                for l0 in range(0, LC, 8):
                    l1 = min(LC, l0 + 8)
                    cx.dma("pool", tc[:, l0:l1, :], tcs[:, l0:l1, :], w=[tc])
                    cx.dma("pool", ts[:, l0:l1, :], tss[:, l0:l1, :], w=[ts])
                for n2 in range(2):
                    pa = self.pa.next()
                    for lc in range(LC):
                        cx.op("pe", lambda lc=lc, n2=n2: nc.tensor.matmul(
                            pa[:, 0:LB], ucb[:, lc, n2 * P:(n2 + 1) * P], tc[:, lc, :],
                            start=(lc == 0), stop=False), r=[ucb, tc], w=[pa])
                    for lc in range(LC):
                        cx.op("pe", lambda lc=lc, n2=n2: nc.tensor.matmul(
                            pa[:, 0:LB], usb[:, lc, n2 * P:(n2 + 1) * P], ts[:, lc, :],
                            start=False, stop=(lc == LC - 1)), r=[usb, ts], w=[pa])
                    o = orr.next()
                    if n2 == 0:
                        cx.op("act", lambda: nc.scalar.copy(out=o[:, 0:LB], in_=pa[:, 0:LB]), r=[pa], w=[o])
                    else:
                        cx.op("dve", lambda: nc.vector.tensor_copy(o[:, 0:LB], pa[:, 0:LB]), r=[pa], w=[o])
                    n0 = nb * 256 + n2 * P
                    c0 = row0 + lb * LB
                    wts = [self.dt(("oT", c0 + i * P)) for i in range(LB // P)]
                    cx.dma("sp", fT[n0:n0 + P, c0:c0 + LB], o[:, 0:LB], r=[o], w=wts)

    def run_pipeline(self, tiles, stages):
        n, S = len(tiles), len(stages)
        state = [dict() for _ in tiles]
        for t in range(n + S - 1):
            for si in range(S - 1, -1, -1):
                i = t - si
                if 0 <= i < n:
                    stages[si](tiles[i][0], tiles[i][1], state[i])

    def rope_tok(self, dst, src, nh, hd0, R, ct, tr):
        cx, nc = self.cx, self.nc
        q4 = R // 4
        sv = src[:, :, hd0:hd0 + R].rearrange("p h (a t j) -> p h a t j", a=2, t=2)
        dv = dst[:, :, hd0:hd0 + R].rearrange("p h (a t j) -> p h a t j", a=2, t=2)
        cs = ct[:, 0:2 * q4].rearrange("p (a j) -> p a j", a=2).unsqueeze(1).to_broadcast([P, nh, 2, q4])
        sn = ct[:, 2 * q4:4 * q4].rearrange("p (a j) -> p a j", a=2).unsqueeze(1).to_broadcast([P, nh, 2, q4])
        x1 = sv[:, :, :, 0, :]
        x2 = sv[:, :, :, 1, :]
        n = nh * 2 * q4

        def tv(t):
            return t[:, 0:n].rearrange("p (h a j) -> p h a j", h=nh, a=2)
        t1, t2, t3, t4 = tr.next(), tr.next(), tr.next(), tr.next()
        cx.op("dve", lambda: nc.vector.tensor_tensor(out=tv(t1), in0=x1, in1=cs, op=ALU.mult), r=[self._rs, ct], w=[t1])
        cx.op("pool", lambda: nc.gpsimd.tensor_tensor(out=tv(t2), in0=x2, in1=sn, op=ALU.mult), r=[self._rs, ct], w=[t2])
        cx.op("dve", lambda: nc.vector.tensor_tensor(out=tv(t3), in0=x2, in1=cs, op=ALU.mult), r=[self._rs, ct], w=[t3])
        cx.op("pool", lambda: nc.gpsimd.tensor_tensor(out=tv(t4), in0=x1, in1=sn, op=ALU.mult), r=[self._rs, ct], w=[t4])
        cx.op("dve", lambda: nc.vector.tensor_tensor(out=dv[:, :, :, 0, :], in0=tv(t1), in1=tv(t2), op=ALU.subtract),
              r=[t1, t2], w=[self._rd])
        cx.op("pool", lambda: nc.gpsimd.tensor_tensor(out=dv[:, :, :, 1, :], in0=tv(t3), in1=tv(t4), op=ALU.add),
              r=[t3, t4], w=[self._rd])

    def tok_to_featT(self, src, src_t, chunks, dst, dst_t, col0, stg_ring):
        cx, nc = self.cx, self.nc
        w = chunks[0][1]
        for i0 in range(0, len(chunks), 4):
            grp = chunks[i0:i0 + 4]
            ng = len(grp)
            pa = self.pa.next()
            for k, (c0, _) in enumerate(grp):
                cx.op("pe", lambda k=k, c0=c0: nc.tensor.transpose(
                    out=pa[0:w, k * P:(k + 1) * P], in_=src[:, c0:c0 + w], identity=self.ident[:]),
                    r=[src_t, self.ident], w=[pa])
            stg = stg_ring.next()
            cx.op("dve", lambda ng=ng: nc.vector.tensor_copy(stg[0:w, 0:ng * P], pa[0:w, 0:ng * P]),
                  r=[pa], w=[stg])
            dv = dst[i0 * w:(i0 + ng) * w, col0:col0 + P].rearrange("(c d) t -> d c t", d=w)
            cx.dma("sp", dv, stg[0:w, 0:ng * P].rearrange("d (c t) -> d c t", c=ng), r=[stg], w=[dst_t])

    def stage_attn(self, H, kvmap, dk, QT, KT, V, q0, nq, k0, nk, scale, oT):
        cx, nc = self.cx, self.nc
        cx.barrier()
        cx.arena_reset()
        NKC = nk // P
        kTb = cx.sba([P, nk], BF16, "kT")
        cx.op("pool", lambda: nc.gpsimd.memset(kTb[:], 0.0), w=[kTb])
        Sh = cx.sba([P, P], F32, "Sh")
        cx.op("dve", lambda: nc.vector.memset(Sh[:], 0.0), w=[Sh])
        cx.op("dve", lambda: nc.vector.tensor_copy(Sh[:, 0:64], self.ident[:, 64:128]), r=[self.ident], w=[Sh])
        Vb = cx.sba([P, NKC, P], BF16, "Vb")
        cx.op("dve", lambda: nc.vector.memset(Vb[:], 1.0), w=[Vb])
        qr = Ring([cx.sba([P, 512], BF16, "qT%d" % i) for i in range(2)])
        for qb_ in qr.bufs:
            cx.op("pool", lambda qb_=qb_: nc.gpsimd.memset(qb_[:], 0.0), w=[qb_])
        pr = Ring([cx.sba([P, 512], BF16, "pT%d" % i) for i in range(4)])
        rsr = Ring([cx.sba([P, 512], F32, "rs%d" % i) for i in range(2)])
        for rb_ in rsr.bufs:
            cx.op("dve", lambda rb_=rb_: nc.vector.memset(rb_[:], 0.0), w=[rb_])
        rhr = Ring([cx.sba([64, 512], F32, "rh%d" % i) for i in range(2)])
        orr = Ring([cx.sba([64, 512], F32, "o%d" % i) for i in range(2)])
        Hkv = max(kvmap) + 1
        ktk = [self.dt(("KT", k0 + i * P)) for i in range(NKC)]
        vtk = [self.dt(("V", k0 + i * P)) for i in range(NKC)]
        for kvh in range(Hkv):
            cx.dma("pool", kTb[0:dk, :], KT[kvh * dk:(kvh + 1) * dk, k0:k0 + nk], r=ktk, w=[kTb])
            vsrc = V[k0:k0 + nk, kvh * 64:(kvh + 1) * 64].rearrange("(c p) d -> p c d", p=P)
            for c0 in range(0, NKC, 8):
                c1 = min(NKC, c0 + 8)
                cx.dma("pool", Vb[:, c0:c1, 0:64], vsrc[:, c0:c1, :], r=vtk, w=[Vb])
            for h in [hh for hh in range(H) if kvmap[hh] == kvh]:
                for qb in range(0, nq, 512):
                    N = min(512, nq - qb)
                    qT = qr.next()
                    qtk = [self.dt(("QT", q0 + qb + i * P)) for i in range(N // P)]
                    cx.dma("pool", qT[0:dk, 0:N], QT[h * dk:(h + 1) * dk, q0 + qb:q0 + qb + N], r=qtk, w=[qT])
                    py = self.py.next()
                    pend = []

                    def pv(kc, pT):
                        cx.op("pe", lambda: nc.tensor.matmul(
                            py[:, 0:N], Vb[:, kc, :], pT[:, 0:N], start=(kc == 0), stop=(kc == NKC - 1)),
                            r=[Vb, pT], w=[py])
                    for kc in range(NKC):
                        ps = self.pa.next()
                        cx.op("pe", lambda kc=kc: nc.tensor.matmul(
                            ps[:, 0:N], kTb[:, kc * P:(kc + 1) * P], qT[:, 0:N], start=True, stop=True),
                            r=[kTb, qT], w=[ps])
                        pT = pr.next()
                        cx.op("act", lambda: nc.scalar.activation(
                            out=pT[:, 0:N], in_=ps[:, 0:N], func=AF.Exp, scale=float(scale)), r=[ps], w=[pT])
                        pend.append((kc, pT))
                        if len(pend) > 2:
                            pv(*pend.pop(0))
                    while pend:
                        pv(*pend.pop(0))
                    rs = rsr.next()
                    cx.op("dve", lambda: nc.vector.reciprocal(out=rs[64:128, 0:N], in_=py[64:128, 0:N]),
                          r=[py], w=[rs])
                    cx.op("pe", lambda: nc.tensor.matmul(
                        py[:, 512:512 + N], Sh[:], rs[:, 0:N], start=True, stop=True),
                        r=[Sh, rs], w=[py])
                    rh = rhr.next()
                    cx.op("act", lambda: nc.scalar.activation(
                        out=rh[:, 0:N], in_=py[0:64, 512:512 + N], func=AF.Identity), r=[py], w=[rh])
                    o = orr.next()
                    cx.op("dve", lambda: nc.vector.tensor_tensor(
                        out=o[:, 0:N], in0=py[0:64, 0:N], in1=rh[:, 0:N], op=ALU.mult), r=[py, rh], w=[o])
                    otk = [self.dt(("oT", q0 + qb + i * P)) for i in range(N // P)]
                    cx.dma("sp", oT[h * 64:(h + 1) * 64, q0 + qb:q0 + qb + N], o[:, 0:N], r=[o], w=otk)

    def setup_gqa(self):
        self.gqa_w_qkv_d = self.inp("gqa_w_qkv", [1, D, 1536])
        self.gqa_w_o_d = self.inp("gqa_w_o", [1, D, D])
        self.gqa_g_d = self.inp("gqa_g", [1, 2, 64])
        self.ropeG_d = self.inp("ropeG", [SEQ, 64])

    def stage_gqa_proj(self, j, tiles, res, QT, KT, V):
        cx, nc = self.cx, self.nc
        cx.barrier()
        cx.arena_reset()
        W = cx.sba([P, KC, 1536], BF16, "Wqkv")
        cx.dma("pool", W[:], self.gqa_w_qkv_d[j].rearrange("(kc p) n -> p kc n", p=P), w=[W])
        g2 = cx.sba([P, 2, 64], F32, "g2")
        cx.dma("sp", g2[:].rearrange("p a d -> p (a d)"),
               self.gqa_g_d[j:j + 1].rearrange("o a d -> o (a d)").partition_broadcast(P), w=[g2])
        gqk = cx.sba([P, 20, 64], F32, "gqk")
        cx.op("dve", lambda: nc.vector.tensor_copy(gqk[:, 0:16, :], g2[:, 0:1, :].to_broadcast([P, 16, 64])),
              r=[g2], w=[gqk])
        cx.op("dve", lambda: nc.vector.tensor_copy(gqk[:, 16:20, :], g2[:, 1:2, :].to_broadcast([P, 4, 64])),
              r=[g2], w=[gqk])
        hTr = Ring([cx.sba([P, KC, P], BF16, "hT%d" % i) for i in range(3)])
        xr = Ring([cx.sba([P, D], F32, "x%d" % i) for i in range(2)])
        xnr = Ring([cx.sba([P, D], F32, "xn%d" % i) for i in range(2)])
        sst = Ring([cx.sba([P, 4], F32, "ss%d" % i) for i in range(2)])
        qkr = Ring([cx.sba([P, 1536], F32, "qkv%d" % i) for i in range(3)])
        qnr = Ring([cx.sba([P, 1280], F32, "qn%d" % i) for i in range(4)])
        qrr = Ring([cx.sba([P, 1280], F32, "qr%d" % i) for i in range(3)])
        tr = Ring([cx.sba([P, 640], F32, "tt%d" % i) for i in range(8)])
        s20 = Ring([cx.sba([P, 64], F32, "s20%d" % i) for i in range(2)])
        ctr = Ring([cx.sba([P, 64], F32, "ct%d" % i) for i in range(2)])
        stg = Ring([cx.sba([P, 512], F32, "stg%d" % i) for i in range(8)])
        qrows = [(QT[c * P:(c + 1) * P, :], None) for c in range(8)]
        krows = [(KT[c * P:(c + 1) * P, :], None) for c in range(2)]
        def g0(row0, v, st_):
            hT = hTr.next()
            self.norm_mod_T(res[row0:row0 + P, :], self.dt(("res", row0)), 0, v, hT, hT, 0, xr, xnr, sst)
            st_["hT"] = hT

        def g1(row0, v, st_):
            hT = st_["hT"]
            qkv = qkr.next()
            for b in range(3):
                pa = self.pa.next()
                for kc in range(KC):
                    cx.op("pe", lambda kc=kc, b=b: nc.tensor.matmul(
                        pa[:], hT[:, kc, :], W[:, kc, b * 512:(b + 1) * 512],
                        start=(kc == 0), stop=(kc == KC - 1)), r=[hT, W], w=[pa])
                if b == 1:
                    cx.op("dve", lambda b=b: nc.vector.tensor_copy(qkv[:, b * 512:(b + 1) * 512], pa[:]),
                          r=[pa], w=[qkv])
                else:
                    cx.op("act", lambda b=b: nc.scalar.activation(
                        out=qkv[:, b * 512:(b + 1) * 512], in_=pa[:], func=AF.Identity), r=[pa], w=[qkv])
            cx.dma("sp", V[row0:row0 + P, 0:256], qkv[:, 1280:1536], r=[qkv], w=[self.dt(("V", row0))])
            st_["qkv"] = qkv

        def g2(row0, v, st_):
            qkv = st_["qkv"]
            qn = qnr.next()
            st = s20.next()
            qk3 = qkv[:, 0:1280].rearrange("p (h d) -> p h d", d=64)
            qn3 = qn[:].rearrange("p (h d) -> p h d", d=64)
            cx.op("pool", lambda: nc.gpsimd.tensor_tensor(out=qn3, in0=qk3, in1=qk3, op=ALU.mult), r=[qkv], w=[qn])
            cx.op("dve", lambda: nc.vector.tensor_reduce(out=st[:, 0:20], in_=qn3, axis=AX.X, op=ALU.add),
                  r=[qn], w=[st])
            cx.op("dve", lambda: nc.vector.tensor_scalar(
                out=st[:, 0:20], in0=st[:, 0:20], scalar1=1.0 / 64, scalar2=EPS, op0=ALU.mult, op1=ALU.add),
                r=[st], w=[st])
            cx.op("act", lambda: nc.scalar.sqrt(out=st[:, 0:20], in_=st[:, 0:20]), r=[st], w=[st])
            cx.op("dve", lambda: nc.vector.reciprocal(out=st[:, 32:52], in_=st[:, 0:20]), r=[st], w=[st])
            cx.op("dve", lambda: nc.vector.tensor_tensor(
                out=qn3, in0=qk3, in1=st[:, 32:52].unsqueeze(2).to_broadcast([P, 20, 64]), op=ALU.mult),
                r=[qkv, st], w=[qn])
            cx.op("pool", lambda: nc.gpsimd.tensor_tensor(out=qn3, in0=qn3, in1=gqk[:], op=ALU.mult),
                  r=[qn, gqk], w=[qn])
            st_["qn"] = qn

        def g3(row0, v, st_):
            qn = st_["qn"]
            if v == 0:
                ct = ctr.next()
                cx.dma("sp", ct[:], self.ropeG_d[row0 - CTX:row0 - CTX + P, :], w=[ct])
                qrt = qrr.next()
                self._rs, self._rd = qn, qrt
                self.rope_tok(qrt[:].rearrange("p (h d) -> p h d", d=64),
                              qn[:].rearrange("p (h d) -> p h d", d=64), 20, 0, 64, ct, tr)
                st_["src"] = qrt
            else:
                st_["src"] = qn

        def g4(row0, v, st_):
            src = st_["src"]
            self.tok_to_featT(src, src, [(c * P, P) for c in range(8)], QT[0:1024, :], self.dt(("QT", row0)), row0, stg)
            self.tok_to_featT(src, src, [(c * P, P) for c in range(8, 10)], KT[0:256, :], self.dt(("KT", row0)), row0, stg)

        self.run_pipeline(tiles, [g0, g1, g2, g3, g4])

    def setup_mla(self):
        self.mla_w_in_d = self.inp("mla_w_in", [1, D, 672])
        self.mla_g_qaT_d = self.inp("mla_g_qaT", [1, P, 3])
        self.mla_w_qb_d = self.inp("mla_w_qb", [1, 384, 1536])
        self.mla_g_kvaT_d = self.inp("mla_g_kvaT", [1, P, 2])
        self.mla_w_kvb_d = self.inp("mla_w_kvb", [1, 256, 2048])
        self.mla_w_o_d = self.inp("mla_w_o", [1, D, D])
        self.ropeM_d = self.inp("ropeM", [SEQ, 32])

    def stage_mla_proj(self, j, tiles, res, QT, KT, V):
        cx, nc = self.cx, self.nc
        cx.barrier()
        cx.arena_reset()
        Win = cx.sba([P, KC, 672], BF16, "Win")
        cx.dma("pool", Win[:], self.mla_w_in_d[j].rearrange("(kc p) n -> p kc n", p=P), w=[Win])
        gq = cx.sba([P, 3], F32, "gqa")
        gk = cx.sba([P, 2], F32, "gkva")
        cx.dma("sp", gq[:], self.mla_g_qaT_d[j], w=[gq])
        cx.dma("sp", gk[:], self.mla_g_kvaT_d[j], w=[gk])
        Wq = cx.sba([P, 3, 1536], BF16, "Wq")
        Wk = cx.sba([P, 2, 2048], BF16, "Wk")
        stg_w = cx.sba([P, 2048], F32, "stgw")
        qsrc = self.mla_w_qb_d[j].rearrange("(kc p) n -> p kc n", p=P)
        ksrc = self.mla_w_kvb_d[j].rearrange("(kc p) n -> p kc n", p=P)
        for kc in range(3):
            cx.dma("sp", stg_w[:, 0:1536], qsrc[:, kc, :], w=[stg_w])
            cx.op("dve", lambda kc=kc: nc.vector.tensor_scalar(
                out=Wq[:, kc, :], in0=stg_w[:, 0:1536], scalar1=gq[:, kc:kc + 1], scalar2=None, op0=ALU.mult),
                r=[stg_w, gq], w=[Wq])
        for kc in range(2):
            cx.dma("sp", stg_w[:], ksrc[:, kc, :], w=[stg_w])
            cx.op("dve", lambda kc=kc: nc.vector.tensor_scalar(
                out=Wk[:, kc, :], in0=stg_w[:], scalar1=gk[:, kc:kc + 1], scalar2=None, op0=ALU.mult),
                r=[stg_w, gk], w=[Wk])
        hTr = Ring([cx.sba([P, KC, P], BF16, "hT%d" % i) for i in range(2)])
        xr = Ring([cx.sba([P, D], F32, "x%d" % i) for i in range(2)])
        xnr = Ring([cx.sba([P, D], F32, "xn%d" % i) for i in range(2)])
        sst = Ring([cx.sba([P, 4], F32, "ss%d" % i) for i in range(4)])
        ar = Ring([cx.sba([P, 672], F32, "a%d" % i) for i in range(3)])
        anr = Ring([cx.sba([P, 640], F32, "an%d" % i) for i in range(2)])
        jr = Ring([cx.sba([P, 384], F32, "junk%d" % i) for i in range(1)])
        antr = Ring([cx.sba([P, 5, P], BF16, "anT%d" % i) for i in range(2)])
        qsr = Ring([cx.sba([P, 1536], F32, "q%d" % i) for i in range(3)])
        kvr = Ring([cx.sba([P, 2048], F32, "kv%d" % i) for i in range(3)])
        qfr = Ring([cx.sba([P, 1536], F32, "qf%d" % i) for i in range(2)])
        kfr = Ring([cx.sba([P, 1536], F32, "kf%d" % i) for i in range(2)])
        tr = Ring([cx.sba([P, 256], F32, "tt%d" % i) for i in range(4)])
        kper = Ring([cx.sba([P, 32], F32, "kpe%d" % i) for i in range(2)])
        ctr = Ring([cx.sba([P, 32], F32, "ct%d" % i) for i in range(2)])
        stg = Ring([cx.sba([P, 512], F32, "stg%d" % i) for i in range(8)])
        def stage_a(row0, v):
            hT = hTr.next()
            self.norm_mod_T(res[row0:row0 + P, :], self.dt(("res", row0)), 0, v, hT, hT, 0, xr, xnr, sst)
            a = ar.next()
            p1 = self.pa.next()
            for kc in range(KC):
                cx.op("pe", lambda kc=kc: nc.tensor.matmul(
                    p1[:], hT[:, kc, :], Win[:, kc, 0:512], start=(kc == 0), stop=(kc == KC - 1)),
                    r=[hT, Win], w=[p1])
            p2 = self.pa.next()
            for kc in range(KC):
                cx.op("pe", lambda kc=kc: nc.tensor.matmul(
                    p2[:, 0:160], hT[:, kc, :], Win[:, kc, 512:672], start=(kc == 0), stop=(kc == KC - 1)),
                    r=[hT, Win], w=[p2])
            cx.op("act", lambda: nc.scalar.activation(out=a[:, 0:512], in_=p1[:], func=AF.Identity), r=[p1], w=[a])
            cx.op("dve", lambda: nc.vector.tensor_copy(a[:, 512:672], p2[:, 0:160]), r=[p2], w=[a])
            s1 = sst.next()
            s2 = sst.next()
            junk = jr.next()
            cx.op("dve", lambda: nc.vector.scalar_tensor_tensor(
                out=junk[:, 0:384], in0=a[:, 0:384], scalar=1.0, in1=a[:, 0:384], op0=ALU.mult, op1=ALU.mult,
                accum_out=s1[:, 0:1]), r=[a], w=[junk, s1])
            self.rstd(s1, 384)
            cx.op("dve", lambda: nc.vector.scalar_tensor_tensor(
                out=junk[:, 0:256], in0=a[:, 384:640], scalar=1.0, in1=a[:, 384:640], op0=ALU.mult, op1=ALU.mult,
                accum_out=s2[:, 0:1]), r=[a], w=[junk, s2])
            self.rstd(s2, 256)
            an = anr.next()
            cx.op("act", lambda: nc.scalar.activation(
                out=an[:, 0:384], in_=a[:, 0:384], func=AF.Identity, scale=s1[:, 2:3]), r=[a, s1], w=[an])
            cx.op("dve", lambda: nc.vector.tensor_scalar(
                out=an[:, 384:640], in0=a[:, 384:640], scalar1=s2[:, 2:3], scalar2=None, op0=ALU.mult),
                r=[a, s2], w=[an])
            anT = antr.next()
            pt1 = self.pa.next()
            for c in range(4):
                cx.op("pe", lambda c=c: nc.tensor.transpose(
                    out=pt1[:, c * P:(c + 1) * P], in_=an[:, c * P:(c + 1) * P], identity=self.ident[:]),
                    r=[an, self.ident], w=[pt1])
            pt2 = self.pa.next()
            cx.op("pe", lambda: nc.tensor.transpose(
                out=pt2[:, 0:P], in_=an[:, 4 * P:5 * P], identity=self.ident[:]), r=[an, self.ident], w=[pt2])
            cx.op("act", lambda: nc.scalar.activation(
                out=anT[:, 0:4, :].rearrange("p c t -> p (c t)"), in_=pt1[:], func=AF.Identity), r=[pt1], w=[anT])
            cx.op("dve", lambda: nc.vector.tensor_copy(anT[:, 4, :], pt2[:, 0:P]), r=[pt2], w=[anT])
            q = qsr.next()
            for b in range(3):
                pq = self.pa.next()
                for kc in range(3):
                    cx.op("pe", lambda kc=kc, b=b: nc.tensor.matmul(
                        pq[:], anT[:, kc, :], Wq[:, kc, b * 512:(b + 1) * 512], start=(kc == 0), stop=(kc == 2)),
                        r=[anT, Wq], w=[pq])
                if b == 1:
                    cx.op("dve", lambda b=b: nc.vector.tensor_copy(q[:, b * 512:(b + 1) * 512], pq[:]), r=[pq], w=[q])
                else:
                    cx.op("act", lambda b=b: nc.scalar.activation(
                        out=q[:, b * 512:(b + 1) * 512], in_=pq[:], func=AF.Identity), r=[pq], w=[q])
            kv = kvr.next()
            for b2 in range(2):
                pk = self.py.next()
                for hh in range(2):
                    b = 2 * b2 + hh
                    for kc in range(2):
                        cx.op("pe", lambda kc=kc, b=b, hh=hh: nc.tensor.matmul(
                            pk[:, hh * 512:(hh + 1) * 512], anT[:, 3 + kc, :], Wk[:, kc, b * 512:(b + 1) * 512],
                            start=(kc == 0), stop=(kc == 1)), r=[anT, Wk], w=[pk])
                if b2 == 0:
                    cx.op("dve", lambda b2=b2: nc.vector.tensor_copy(kv[:, b2 * 1024:(b2 + 1) * 1024], pk[:]),
                          r=[pk], w=[kv])
                else:
                    cx.op("act", lambda b2=b2: nc.scalar.activation(
                        out=kv[:, b2 * 1024:(b2 + 1) * 1024], in_=pk[:], func=AF.Identity), r=[pk], w=[kv])
            return a, q, kv

        def stage_b(row0, v, a, q, kv):
            kv3 = kv[:].rearrange("p (h d) -> p h d", d=128)
            q3 = q[:].rearrange("p (h d) -> p h d", d=96)
            cx.dma("sp", V[row0:row0 + P, :].rearrange("p (h d) -> p h d", d=64), kv3[:, :, 64:128],
                   r=[kv], w=[self.dt(("V", row0))])
            kf = kfr.next()
            kf3 = kf[:].rearrange("p (h d) -> p h d", d=96)
            cx.op("pool", lambda: nc.gpsimd.tensor_copy(kf3[:, :, 0:64], kv3[:, :, 0:64]), r=[kv], w=[kf])
            if v == 0:
                ct = ctr.next()
                cx.dma("sp", ct[:], self.ropeM_d[row0 - CTX:row0 - CTX + P, :], w=[ct])
                qf = qfr.next()
                qf3 = qf[:].rearrange("p (h d) -> p h d", d=96)
                cx.op("pool", lambda: nc.gpsimd.tensor_copy(qf3[:, :, 0:64], q3[:, :, 0:64]), r=[q], w=[qf])
                self._rs, self._rd = q, qf
                self.rope_tok(qf3, q3, 16, 64, 32, ct, tr)
                kpe = kper.next()
                self._rs, self._rd = a, kpe
                self.rope_tok(kpe[:].rearrange("p (h d) -> p h d", h=1),
                              a[:, 640:672].rearrange("p (h d) -> p h d", h=1), 1, 0, 32, ct, tr)
                cx.op("dve", lambda: nc.vector.tensor_copy(
                    kf3[:, :, 64:96], kpe[:].unsqueeze(1).to_broadcast([P, 16, 32])), r=[kpe], w=[kf])
                qsrc_t = qf
            else:
                cx.op("dve", lambda: nc.vector.tensor_copy(
                    kf3[:, :, 64:96], a[:, 640:672].unsqueeze(1).to_broadcast([P, 16, 32])), r=[a], w=[kf])
                qsrc_t = q
            self.tok_to_featT(qsrc_t, qsrc_t, [(h * 96, 96) for h in range(16)], QT[0:1536, :],
                              self.dt(("QT", row0)), row0, stg)
            self.tok_to_featT(kf, kf, [(h * 96, 96) for h in range(16)], KT[0:1536, :],
                              self.dt(("KT", row0)), row0, stg)

        pend = None
        for (row0, v) in tiles:
            st_new = stage_a(row0, v)
            if pend is not None:
                stage_b(*pend)
            pend = (row0, v) + st_new
        stage_b(*pend)


def host_common():
    return {"ident": np.eye(P, dtype=np.float32), "ones": np.ones((P, P), np.float32)}


def _featT(v):
    v = np.asarray(v)
    return np.ascontiguousarray(np.swapaxes(v.reshape(v.shape[:-1] + (KC, P)), -1, -2))


def host_mod(c_b, c_ctx, w_mod, b_mod, g_mix, g_ffn):
    cs = np.stack([c_b, c_ctx], axis=-1)
    csT = np.ascontiguousarray(cs.reshape(KC, P, 2).transpose(1, 0, 2))
    b_modT = np.ascontiguousarray(b_mod.reshape(DEPTH, 48, P).transpose(0, 2, 1))
    return {"csT": csT, "w_mod": np.ascontiguousarray(w_mod), "b_modT": b_modT,
            "b_mod": np.ascontiguousarray(b_mod.reshape(DEPTH, 1, 6 * D)),
            "g_mixT": _featT(g_mix), "g_ffnT": _featT(g_ffn)}


def host_moe(w_r, b_r, w_gu, b_gu, w_dn, b_dn):
    nl, ne = w_gu.shape[:2]
    g = w_gu[..., 0::2].reshape(nl, ne, D, KC, P)
    u = w_gu[..., 1::2].reshape(nl, ne, D, KC, P)
    wgu = np.ascontiguousarray(np.stack([g, u], axis=-2).reshape(nl, ne, D, 2 * D))
    bg = b_gu[..., 0::2].reshape(nl, ne, KC, P)
    bu = b_gu[..., 1::2].reshape(nl, ne, KC, P)
    bguT = np.stack([bg, bu], axis=-1)
    bguT = np.ascontiguousarray(bguT.transpose(0, 3, 1, 2, 4).reshape(nl, P, ne, 16))
    return {"wgu": wgu, "bguT": bguT, "wdn": np.ascontiguousarray(w_dn), "bdn": np.ascontiguousarray(b_dn),
            "wr": np.ascontiguousarray(w_r), "br": np.ascontiguousarray(b_r.reshape(nl, 1, ne))}


def host_fnet(f_w_in, f_w_out):
    import ml_dtypes
    k = np.arange(P, dtype=np.float64)
    ang = 2 * np.pi * np.outer(k, k) / P
    csc = np.concatenate([np.cos(ang), np.sin(ang)], axis=1).astype(np.float32)

    def tab(L):
        i = np.arange(L, dtype=np.int64)
        m = (np.outer(i, i) % L).astype(np.float64) * (2 * np.pi / L)
        sc = 1.0 / np.sqrt(L * P)
        return np.stack([np.cos(m) * sc, -np.sin(m) * sc]).astype(np.float32)

    return {"f_w_in": np.ascontiguousarray(f_w_in), "f_w_out": np.ascontiguousarray(f_w_out),
            "csc": csc, "tabL": tab(SEQ), "tabC": tab(CTX)}


def _rope_tab(n_tok, R):
    rows = n_tok // 64
    row = np.repeat(np.arange(rows, dtype=np.float32), 64)
    col = np.tile(np.arange(64, dtype=np.float32), rows)
    nf = R // 4
    inv = (np.float32(10000.0) ** (-np.arange(nf, dtype=np.float32) / nf)).astype(np.float32)
    ang = np.stack([row[:, None] * inv, col[:, None] * inv], axis=1).astype(np.float32)
    return np.concatenate([np.cos(ang).reshape(n_tok, 2 * nf), np.sin(ang).reshape(n_tok, 2 * nf)],
                          axis=1).astype(np.float32)


def host_gqa(w_qkv, g_q, g_k, w_o):
    return {"gqa_w_qkv": np.ascontiguousarray(w_qkv), "gqa_w_o": np.ascontiguousarray(w_o),
            "gqa_g": np.ascontiguousarray(np.stack([g_q, g_k], axis=1)), "ropeG": _rope_tab(SEQ, 64)}


def host_mla(w_in, g_qa, w_qb, g_kva, w_kvb, w_o):
    return {"mla_w_in": np.ascontiguousarray(w_in),
            "mla_g_qaT": np.ascontiguousarray(np.swapaxes(g_qa.reshape(-1, 3, P), 1, 2)),
            "mla_w_qb": np.ascontiguousarray(w_qb),
            "mla_g_kvaT": np.ascontiguousarray(np.swapaxes(g_kva.reshape(-1, 2, P), 1, 2)),
            "mla_w_kvb": np.ascontiguousarray(w_kvb), "mla_w_o": np.ascontiguousarray(w_o),
            "ropeM": _rope_tab(SEQ, 32)}


TT = CTX + SEQ


def build_full():
    p = Prog({})
    cx = p.cx
    p.setup_common()
    p.setup_mod()
    p.setup_fnet()
    p.setup_mla()
    p.setup_gqa()
    p.setup_moe(DEPTH, NE)
    res0_d = p.inp("res0", [TT, D])
    gf_d = p.inp("g_final", [1, D])
    out_d = p.outp("out", [SEQ, D])
    res = p.scratch("res", [TT, D])
    UC = p.scratch("UC", [TT, D])
    US = p.scratch("US", [TT, D])
    oT = p.scratch("oT", [D, TT])
    QT = p.scratch("QT", [1536, TT])
    KT = p.scratch("KT", [1536, TT])
    V = p.scratch("V", [TT, D])
    all_tiles = [(i * P, 1 if i < CTX // P else 0) for i in range(TT // P)]
    lat_tiles = [t for t in all_tiles if t[1] == 0]
    for (r0, v) in all_tiles:
        cx.dma("sp", res[r0:r0 + P, :], res0_d[r0:r0 + P, :], w=[p.dt(("res", r0))])
    p.stage_mod(0)
    p.stage_fnet1(0, all_tiles, res, UC, US)
    p.stage_fnet2(CTX, 0, p.tabC_d, UC, US, oT)
    p.stage_fnet2(SEQ, CTX, p.tabL_d, UC, US, oT)
    p.stage_outproj(all_tiles, res, oT, p.f_w_out_d[0])
    p.stage_moe(0, all_tiles, res, NE, sb_sizes=[10, 8, 8, 8])
    p.stage_mod(1)
    p.stage_mla_proj(0, all_tiles, res, QT, KT, V)
    p.stage_attn(16, list(range(16)), 96, QT, KT, V, CTX, SEQ, 0, TT, 96 ** -0.5, oT)
    p.stage_attn(16, list(range(16)), 96, QT, KT, V, 0, CTX, 0, CTX, 96 ** -0.5, oT)
    p.stage_outproj(all_tiles, res, oT, p.mla_w_o_d[0])
    p.stage_moe(1, all_tiles, res, NE, sb_sizes=[10, 8, 8, 8])
    p.stage_mod(2)
    p.stage_gqa_proj(0, all_tiles, res, QT, KT, V)
    kvmap = [h // 4 for h in range(16)]
    p.stage_attn(16, kvmap, 64, QT, KT, V, CTX, SEQ, 0, TT, 64 ** -0.5, oT)
    p.stage_outproj(lat_tiles, res, oT, p.gqa_w_o_d[0])
    p.stage_moe(2, lat_tiles, res, NE, nsb_tiles=8)
    p.stage_mod(3)
    p.stage_fnet1(1, lat_tiles, res, UC, US)
    p.stage_fnet2(SEQ, CTX, p.tabL_d, UC, US, oT)
    p.stage_outproj(lat_tiles, res, oT, p.f_w_out_d[1])
    p.stage_moe(3, lat_tiles, res, NE, final=(gf_d, out_d, CTX), nsb_tiles=8)
    cx.finish()
    return p


_PROG = None


def kernel(x, c, ctx, c_ctx, w_mod, b_mod, g_mix, g_ffn, g_final, f_w_in, f_w_out,
           mla_w_in, mla_g_qa, mla_w_qb, mla_g_kva, mla_w_kvb, mla_w_o,
           gqa_w_qkv, gqa_g_q, gqa_g_k, gqa_w_o,
           moe_w_router, moe_b_router, moe_w_gu, moe_b_gu, moe_w_down, moe_b_down):
    global _PROG
    f = lambda a: np.asarray(a, dtype=np.float32)
    x, c, ctx, c_ctx = f(x), f(c), f(ctx), f(c_ctx)
    if _PROG is None:
        _PROG = build_full()
    p = _PROG
    shared = host_common()
    shared.update(host_fnet(f(f_w_in), f(f_w_out)))
    shared.update(host_mla(f(mla_w_in), f(mla_g_qa), f(mla_w_qb), f(mla_g_kva), f(mla_w_kvb), f(mla_w_o)))
    shared.update(host_gqa(f(gqa_w_qkv), f(gqa_g_q), f(gqa_g_k), f(gqa_w_o)))
    shared.update(host_moe(f(moe_w_router), f(moe_b_router), f(moe_w_gu), f(moe_b_gu), f(moe_w_down), f(moe_b_down)))
    shared["g_final"] = np.ascontiguousarray(f(g_final).reshape(1, D))
    nb = x.shape[0]
    in_maps = []
    for b in range(nb):
        m = dict(shared)
        m.update(host_mod(c[b], c_ctx, f(w_mod), f(b_mod), f(g_mix), f(g_ffn)))
        m["res0"] = np.ascontiguousarray(np.concatenate([ctx[b], x[b]], axis=0))
        in_maps.append(m)
    r = run_bass_kernel_spmd(p.nc, in_maps, core_ids=list(range(nb)))
    return np.stack([np.asarray(r.results[b]["out"], dtype=np.float32) for b in range(nb)], axis=0)
```
